# Optimizing a Trainium2 kernel written in Bass

```python
import math
import jax, jax.numpy as jnp
from jax import lax
import numpy as np

D_MODEL = 1024
BATCH = 8
SEQ = 4096
DEPTH = 4

GRID_W = 64
CTX_LEN = 256
N_MIXERS = 3
N_MOD = 6
CONV_WIDTH = 31
CONV_PAD = (CONV_WIDTH - 1) // 2
F_GROUPS = 8
F_GROUP_DIM = D_MODEL // F_GROUPS
DA_HEADS = 8
DA_QK_DIM = D_MODEL // DA_HEADS // 2
DA_V_DIM = 2 * DA_QK_DIM
ROPE_AXIS_DIM = DA_QK_DIM // 2
ROPE_FREQS = ROPE_AXIS_DIM // 2
ROPE_BASE = 10000.0
Q_BLOCK = 128
D_FF = 3584
N_EXPERTS = 8
TOP_K = 2
NORM_EPS = 1e-6
N_CONV = (DEPTH + 2) // 3
N_FOURIER = (DEPTH + 1) // 3
N_ATTN = DEPTH // 3
N_DENSE = (DEPTH + 1) // 2
N_MOE = DEPTH // 2

kernel_name = 'hybrid_conv_fnet_diffattn_moe_dit'


def rms_norm(x, g, eps=NORM_EPS):
    xf = x.astype(jnp.float32)
    y = xf * lax.rsqrt(jnp.mean(jnp.square(xf), axis=-1, keepdims=True) + eps)
    return (y * g.astype(jnp.float32)).astype(x.dtype)


def layer_norm(x, g, b, eps=1e-5):
    xf = x.astype(jnp.float32)
    mu = jnp.mean(xf, axis=-1, keepdims=True)
    var = jnp.mean(jnp.square(xf - mu), axis=-1, keepdims=True)
    y = (xf - mu) * lax.rsqrt(var + eps)
    return (y * g.astype(jnp.float32) + b.astype(jnp.float32)).astype(x.dtype)


def axial_rope_tables(n_tokens, dtype):
    rows = n_tokens // GRID_W
    row = jnp.repeat(jnp.arange(rows, dtype=jnp.float32), GRID_W)
    col = jnp.tile(jnp.arange(GRID_W, dtype=jnp.float32), rows)
    inv_freq = 1.0 / (ROPE_BASE ** (jnp.arange(ROPE_FREQS, dtype=jnp.float32) * 2.0 / ROPE_AXIS_DIM))
    ang = jnp.stack([row[:, None] * inv_freq, col[:, None] * inv_freq], axis=1)
    ang = jnp.stack([ang, ang], axis=2).reshape(n_tokens, DA_QK_DIM)
    return jnp.cos(ang).astype(dtype), jnp.sin(ang).astype(dtype)


def rope_2d(x, cos, sin):
    xs = x.reshape(x.shape[:-1] + (2, 2, ROPE_FREQS))
    rot = jnp.stack([-xs[..., 1, :], xs[..., 0, :]], axis=-2).reshape(x.shape)
    return x * cos + rot * sin


def conformer_conv(h, w_in, b_in, w_dw, b_dw, ln_g, ln_b, w_out, b_out):
    a, g = jnp.split(h @ w_in + b_in, 2, axis=-1)
    u = a * jax.nn.sigmoid(g)
    u = lax.conv_general_dilated(u, w_dw[:, None, :], window_strides=(1,),
                                 padding=[(CONV_PAD, CONV_PAD)],
                                 dimension_numbers=('NWC', 'WIO', 'NWC'),
                                 feature_group_count=D_MODEL) + b_dw
    u = jax.nn.silu(layer_norm(u, ln_g, ln_b))
    return u @ w_out + b_out


def fourier_mix(h, w_f, b_f):
    b_, n, _ = h.shape
    hg = h.astype(jnp.float32).reshape(b_, n, F_GROUPS, F_GROUP_DIM)
    f = jnp.fft.fft2(hg, axes=(1, 3), norm='ortho').real.astype(h.dtype)
    return f.reshape(b_, n, D_MODEL) @ w_f + b_f


def diff_attn_core(q, k, v, lam):
    s = jnp.einsum('bqhmd,bkhmd->bhmqk', q, k).astype(jnp.float32) * (DA_QK_DIM ** -0.5)
    p = jax.nn.softmax(s, axis=-1)
    a = p[:, :, 0] - lam * p[:, :, 1]
    return jnp.einsum('bhqk,bkhd->bqhd', a.astype(v.dtype), v)


def diff_attention(hx, hc, w_qkv, lam_vecs, subln_g, w_o, lam_init, need_ctx):
    b_, n, _ = hx.shape

    def project(h):
        q, k, v = jnp.split(h @ w_qkv, 3, axis=-1)
        m = h.shape[1]
        return (q.reshape(b_, m, DA_HEADS, 2, DA_QK_DIM),
                k.reshape(b_, m, DA_HEADS, 2, DA_QK_DIM),
                v.reshape(b_, m, DA_HEADS, DA_V_DIM))

    qx, kx, vx = project(hx)
    qc, kc, vc = project(hc)
    cos, sin = axial_rope_tables(n, hx.dtype)
    cos = cos[None, :, None, None, :]
    sin = sin[None, :, None, None, :]
    qx = rope_2d(qx, cos, sin)
    kx = rope_2d(kx, cos, sin)
    lv = lam_vecs.astype(jnp.float32)
    lam = jnp.exp(jnp.sum(lv[0] * lv[1])) - jnp.exp(jnp.sum(lv[2] * lv[3])) + lam_init

    k_all = jnp.concatenate([kc, kx], axis=1)
    v_all = jnp.concatenate([vc, vx], axis=1)
    n_blk = n // Q_BLOCK
    qb = qx.reshape(b_, n_blk, Q_BLOCK, DA_HEADS, 2, DA_QK_DIM).swapaxes(0, 1)
    ox = lax.map(lambda qblk: diff_attn_core(qblk, k_all, v_all, lam), qb)
    ox = ox.swapaxes(0, 1).reshape(b_, n, DA_HEADS, DA_V_DIM)

    def finish(o):
        o = rms_norm(o, subln_g) * (1.0 - lam_init)
        return o.reshape(o.shape[0], o.shape[1], D_MODEL) @ w_o

    out_x = finish(ox)
    out_c = finish(diff_attn_core(qc, kc, vc, lam)) if need_ctx else None
    return out_x, out_c


def swiglu(h, w_gate, w_up, w_down):
    return (jax.nn.silu(h @ w_gate) * (h @ w_up)) @ w_down


def moe_swiglu(h, w_router, w_gate, w_up, w_down):
    b_, n, d = h.shape
    t = h.reshape(-1, d)
    logits = (t @ w_router).astype(jnp.float32)
    top_val, top_idx = lax.top_k(logits, TOP_K)
    top_w = jax.nn.softmax(top_val, axis=-1)
    gates = jnp.sum(jax.nn.one_hot(top_idx, N_EXPERTS, dtype=jnp.float32) * top_w[..., None],
                    axis=1).astype(h.dtype)
    y = jnp.zeros_like(t)
    for e in range(N_EXPERTS):
        y = y + gates[:, e:e + 1] * swiglu(t, w_gate[e], w_up[e], w_down[e])
    return y.reshape(b_, n, d)


def setup_inputs(seed: int = 0) -> dict:
    key = jax.random.key(seed)
    ks = iter(jax.random.split(key, 32))
    D = D_MODEL

    def nrm(shape, scale):
        return jax.random.normal(next(ks), shape, jnp.float32) * scale

    return {
        'x': nrm((BATCH, SEQ, D), 1.0),
        'c': nrm((BATCH, D), 1.0),
        'ctx': nrm((BATCH, CTX_LEN, D), 1.0),
        'c_ctx': nrm((D,), 1.0),
        'w_mod': nrm((DEPTH, D, N_MOD * D), 0.5 * D ** -0.5),
        'b_mod': nrm((DEPTH, N_MOD * D), 0.01),
        'norm_g': 1.0 + nrm((DEPTH, 2, D), 0.05),
        'conv_w_in': nrm((N_CONV, D, 2 * D), D ** -0.5),
        'conv_b_in': nrm((N_CONV, 2 * D), 0.01),
        'conv_w_dw': nrm((N_CONV, CONV_WIDTH, D), CONV_WIDTH ** -0.5),
        'conv_b_dw': nrm((N_CONV, D), 0.01),
        'conv_ln_g': 1.0 + nrm((N_CONV, D), 0.05),
        'conv_ln_b': nrm((N_CONV, D), 0.01),
        'conv_w_out': nrm((N_CONV, D, D), D ** -0.5),
        'conv_b_out': nrm((N_CONV, D), 0.01),
        'fnet_w': nrm((N_FOURIER, D, D), D ** -0.5),
        'fnet_b': nrm((N_FOURIER, D), 0.01),
        'attn_w_qkv': nrm((N_ATTN, D, 3 * D), D ** -0.5),
        'attn_lambda': nrm((N_ATTN, 4, DA_QK_DIM), 0.1),
        'attn_subln_g': 1.0 + nrm((N_ATTN, DA_V_DIM), 0.05),
        'attn_w_o': nrm((N_ATTN, D, D), D ** -0.5),
        'ffn_w_gate': nrm((N_DENSE, D, D_FF), D ** -0.5),
        'ffn_w_up': nrm((N_DENSE, D, D_FF), D ** -0.5),
        'ffn_w_down': nrm((N_DENSE, D_FF, D), D_FF ** -0.5),
        'moe_w_router': nrm((N_MOE, D, N_EXPERTS), D ** -0.5),
        'moe_w_gate': nrm((N_MOE, N_EXPERTS, D, D_FF), D ** -0.5),
        'moe_w_up': nrm((N_MOE, N_EXPERTS, D, D_FF), D ** -0.5),
        'moe_w_down': nrm((N_MOE, N_EXPERTS, D_FF, D), D_FF ** -0.5),
        'final_g': 1.0 + nrm((D,), 0.05),
    }


def reference(x, c, ctx, c_ctx, w_mod, b_mod, norm_g,
              conv_w_in, conv_b_in, conv_w_dw, conv_b_dw, conv_ln_g, conv_ln_b, conv_w_out, conv_b_out,
              fnet_w, fnet_b, attn_w_qkv, attn_lambda, attn_subln_g, attn_w_o,
              ffn_w_gate, ffn_w_up, ffn_w_down,
              moe_w_router, moe_w_gate, moe_w_up, moe_w_down, final_g):
    b_, n, d = x.shape
    n_ctx = ctx.shape[1]
    cx = ctx
    sc = jax.nn.silu(c)
    scc = jax.nn.silu(c_ctx)
    for i in range(DEPTH):
        need_ctx = i < DEPTH - 1
        kind = i % N_MIXERS
        mx = (sc @ w_mod[i] + b_mod[i]).reshape(b_, N_MOD, 1, d)
        mc = (scc @ w_mod[i] + b_mod[i]).reshape(N_MOD, d)

        hx = rms_norm(x, norm_g[i, 0]) * (1 + mx[:, 1]) + mx[:, 0]
        if need_ctx or kind == 2:
            hc = rms_norm(cx, norm_g[i, 0]) * (1 + mc[1]) + mc[0]
        j = i // N_MIXERS
        if kind == 0:
            p = (conv_w_in[j], conv_b_in[j], conv_w_dw[j], conv_b_dw[j],
                 conv_ln_g[j], conv_ln_b[j], conv_w_out[j], conv_b_out[j])
            ox = conformer_conv(hx, *p)
            oc = conformer_conv(hc, *p) if need_ctx else None
        elif kind == 1:
            ox = fourier_mix(hx, fnet_w[j], fnet_b[j])
            oc = fourier_mix(hc, fnet_w[j], fnet_b[j]) if need_ctx else None
        else:
            lam_init = 0.8 - 0.6 * math.exp(-0.3 * i)
            ox, oc = diff_attention(hx, hc, attn_w_qkv[j], attn_lambda[j], attn_subln_g[j],
                                    attn_w_o[j], lam_init, need_ctx)
        x = x + mx[:, 2] * ox
        if need_ctx:
            cx = cx + mc[2] * oc

        hx = rms_norm(x, norm_g[i, 1]) * (1 + mx[:, 4]) + mx[:, 3]
        if need_ctx:
            hc = rms_norm(cx, norm_g[i, 1]) * (1 + mc[4]) + mc[3]
            h = jnp.concatenate([hc, hx], axis=1)
        else:
            h = hx
        j = i // 2
        if i % 2 == 0:
            o = swiglu(h, ffn_w_gate[j], ffn_w_up[j], ffn_w_down[j])
        else:
            o = moe_swiglu(h, moe_w_router[j], moe_w_gate[j], moe_w_up[j], moe_w_down[j])
        x = x + mx[:, 5] * o[:, h.shape[1] - n:]
        if need_ctx:
            cx = cx + mc[5] * o[:, :n_ctx]
    return rms_norm(x, final_g)
```

```python
import contextlib
import numpy as np
import ml_dtypes
import concourse.bass as bass
import concourse.mybir as mybir
from concourse.bass_utils import run_bass_kernel_spmd

F32 = mybir.dt.float32
BF16 = mybir.dt.bfloat16
AF = mybir.ActivationFunctionType
ALU = mybir.AluOpType
AX = mybir.AxisListType

D = 1024
NCTX = 256
NX = 4096
NTOK = NCTX + NX
NT = NTOK // 128
DFF = 3584
NE = 8
SEM_EPOCH = 4000
COMPUTE = ("pe", "act", "dve", "pool")
ENGMAP = {"pe": "pe", "act": "act", "dve": "dve", "pool": "pool", "sp": "sp", "actq": "act", "poolq": "pool"}


class Buf:
    __slots__ = ("name", "last_w", "readers")

    def __init__(self, name=""):
        self.name = name
        self.last_w = None
        self.readers = []


class Op:
    __slots__ = ("eng", "fn", "reads", "writes", "ndma", "key", "deps", "idx", "needed", "ev", "xdeps")

    def __init__(self, eng, fn, reads, writes, ndma, key, xdeps):
        self.eng = eng
        self.fn = fn
        self.reads = reads
        self.writes = writes
        self.ndma = ndma
        self.key = key
        self.deps = []
        self.needed = False
        self.ev = None
        self.xdeps = xdeps


class Prog:
    def __init__(self):
        self.ops = []
        self.last_real = {}
        self.dma_since = []

    def op(self, eng, fn, reads=(), writes=(), ndma=0, key=None, xdeps=()):
        o = Op(eng, fn, tuple(reads), tuple(writes), ndma, key, tuple(xdeps))
        o.idx = len(self.ops)
        self.ops.append(o)
        if fn is not None:
            self.last_real[ENGMAP[eng]] = o
            if ndma:
                self.dma_since.append(o)
        return o

    def chain(self, eng, fns, reads=(), writes=()):
        cb = Buf()
        for f in fns:
            self.op(eng, f, reads=tuple(reads) + (cb,), writes=tuple(writes) + (cb,))

    def barrier(self):
        deps = list(self.last_real.values()) + list(self.dma_since)
        self.dma_since = []
        for s in ("pe", "act", "dve", "pool", "sp"):
            self.op(s, None, xdeps=deps)

    def analyze(self):
        for o in self.ops:
            deps = {}
            for b in o.reads:
                if b.last_w is not None:
                    deps[b.last_w.idx] = b.last_w
            for b in o.writes:
                if b.last_w is not None:
                    deps[b.last_w.idx] = b.last_w
                for r in b.readers:
                    deps[r.idx] = r
            for d in o.xdeps:
                deps[d.idx] = d
            deps.pop(o.idx, None)
            o.deps = list(deps.values())
            for b in o.reads:
                b.readers.append(o)
            for b in o.writes:
                b.last_w = o
                b.readers = []

    def emit(self, nc, final_wait_keys=()):
        self.analyze()
        streams = {"pe": [], "act": [], "dve": [], "pool": [], "sp": []}
        for o in self.ops:
            streams[ENGMAP[o.eng]].append(o)
        for o in self.ops:
            for d in o.deps:
                if d.eng == "pe" and o.eng == "pe":
                    continue
                d.needed = True
        ccount = {e: 0 for e in COMPUTE}
        dcount = {}
        sem_names = []
        for o in self.ops:
            if o.fn is None:
                continue
            if o.ndma:
                k = o.key
                dcount[k] = dcount.get(k, 0) + 16 * o.ndma
                o.ev = ("d:" + k, dcount[k])
            elif o.needed:
                e = o.eng
                ccount[e] += 1
                ep = (ccount[e] - 1) // SEM_EPOCH
                o.ev = ("c:%s:%d" % (e, ep), ccount[e] - ep * SEM_EPOCH)
            if o.ev is not None and o.ev[0] not in sem_names:
                sem_names.append(o.ev[0])
        self.final = dict(dcount)
        self.nsem = len(sem_names)
        with contextlib.ExitStack() as es:
            sems = {n: es.enter_context(nc.semaphore(n.replace(":", "_"))) for n in sem_names}
            block = es.enter_context(nc.Block())
            latest = {}
            waits_for = {}
            seen = {s: {} for s in streams}
            for o in self.ops:
                need = {}
                for d in o.deps:
                    if d.eng == "pe" and o.eng == "pe":
                        continue
                    sname, val = d.ev
                    if sname[0] == "d":
                        val = max(val, latest.get(sname, 0))
                    if need.get(sname, 0) < val:
                        need[sname] = val
                st = seen[ENGMAP[o.eng]]
                w = []
                for sname, val in need.items():
                    if st.get(sname, 0) >= val:
                        continue
                    st[sname] = val
                    w.append((sname, val))
                waits_for[o.idx] = w
                if o.ev is not None and o.ndma:
                    latest[o.ev[0]] = o.ev[1]

            def make(stream_name):
                ops = streams[stream_name]

                def body(eng):
                    for o in ops:
                        for sname, val in waits_for[o.idx]:
                            eng.wait_ge(sems[sname], val)
                        if o.fn is None:
                            continue
                        r = o.fn(eng)
                        if o.ndma:
                            assert len(r) == o.ndma, (len(r), o.ndma, o.key)
                            for ins in r:
                                ins.then_inc(sems[o.ev[0]], 16)
                        elif o.ev is not None:
                            r.then_inc(sems[o.ev[0]], 1)
                    if stream_name == "sp":
                        for k in final_wait_keys:
                            eng.wait_ge(sems["d:" + k], self.final[k])
                return body

            block.tensor(make("pe"))
            block.scalar(make("act"))
            block.vector(make("dve"))
            block.gpsimd(make("pool"))
            block.sync(make("sp"))


SB_BASE = 16512
SB_LIMIT = 229344


class KB:
    def __init__(self):
        self.nc = bass.Bass("TRN2", target_bir_lowering=False)
        self.P = Prog()
        self.sb_off = SB_BASE
        self.sb_mark = SB_BASE
        self.uid = 0
        self.ins = {}
        self.outs = {}
        self.psb = []
        self.pbuf = []

    def din(self, name, shape, dt=F32):
        t = self.nc.dram_tensor(name, list(shape), dt, kind="ExternalInput")
        self.ins[name] = (tuple(shape), dt)
        return t

    def dout(self, name, shape, dt=F32):
        t = self.nc.dram_tensor(name, list(shape), dt, kind="ExternalOutput")
        self.outs[name] = (tuple(shape), dt)
        return t

    def dscr(self, name, shape, dt=F32):
        return self.nc.dram_tensor(name, list(shape), dt, kind="Internal")

    def sb(self, name, shape, dt):
        esz = 4 if dt == F32 else 2
        n = 1
        for s in shape[1:]:
            n *= s
        nbytes = (n * esz + 63) // 64 * 64
        off = self.sb_off
        assert off + nbytes <= SB_LIMIT, ("SBUF overflow", name, off, nbytes)
        self.sb_off += nbytes
        self.uid += 1
        return self.nc.alloc_sbuf_tensor_at("%s_%d" % (name, self.uid), list(shape), dt, offset=off)

    def persist(self):
        self.sb_mark = self.sb_off

    def stage_reset(self):
        self.P.barrier()
        self.sb_off = self.sb_mark

    def init_psum(self):
        for i in range(8):
            self.psb.append(self.nc.alloc_psum_tensor("psb%d" % i, [128, 512], F32))
            self.pbuf.append(Buf("ps%d" % i))


def bcast_rows(t, off, n):
    return bass.AP(t, off, [[0, 128], [1, n]])


class Common:
    pass


def setup_common(K, dr):
    P = K.P
    C = Common()
    C.ident = K.sb("ident", [128, 128], F32)
    C.b_ident = Buf()
    P.op("sp", lambda e: [e.dma_start(out=C.ident[:], in_=dr["ident"].ap())], writes=[C.b_ident], ndma=1, key="ident")
    C.eps = K.sb("eps", [128, 1], F32)
    C.b_eps = Buf()
    P.op("dve", lambda e: e.memset(C.eps[:], 1e-6), writes=[C.b_eps])
    C.ones = K.sb("ones", [128, 128], F32)
    C.b_ones = Buf()
    P.op("dve", lambda e: e.memset(C.ones[:], 1.0), writes=[C.b_ones])
    C.pcol = K.sb("pcol", [128, dr["pcol_n"]], F32)
    C.b_pcol = Buf()
    P.op("sp", lambda e: [e.dma_start(out=C.pcol[:], in_=dr["pcol"].ap())], writes=[C.b_pcol], ndma=1, key="pcol")
    C.modcol = K.sb("modcol", [128, 4 * 96], F32)
    C.b_modcol = Buf()
    C.scol = K.sb("scol", [128, 4 * 2 * 2 * 8], F32)
    C.b_scol = Buf()
    C.bcol = K.sb("bcol", [128, 4 * 2 * 2 * 8], F32)
    return C


def stage_mod(K, C, dr):
    P = K.P
    nc = K.nc
    ccol = K.sb("ccol", [128, 16], F32)
    sc = K.sb("sc", [128, 16], F32)
    b_cc, b_sc = Buf(), Buf()
    P.op("sp", lambda e: [e.dma_start(out=ccol[:], in_=dr["ccol"].ap())], writes=[b_cc], ndma=1, key="ccol")
    P.op("act", lambda e: e.activation(out=sc[:], in_=ccol[:], func=AF.Silu), reads=[b_cc], writes=[b_sc])
    wm = [K.sb("wm%d" % i, [128, 6144], F32) for i in range(2)]
    b_wm = [Buf(), Buf()]
    acc_col = K.sb("acc_col", [128, 96], F32)
    acc_row = K.sb("acc_row", [2, 2048], F32)
    brow = K.sb("brow", [2, 2048], F32)
    bmc = K.sb("bmc", [128, 48], F32)
    b_acc_col, b_acc_row, b_brow, b_bmc = Buf(), Buf(), Buf(), Buf()
    pcolv = K.psb[0]
    prow = [K.psb[1 + j] for j in range(4)]
    w_mod = dr["w_mod"]
    it = 0
    for i in range(4):
        P.op("sp", lambda e, i=i: [e.dma_start(out=bmc[:], in_=dr["bmodcol"].ap()[i])], writes=[b_bmc], ndma=1, key="bmc")

        def ldb(e, i=i):
            r = []
            for xc in range(2):
                for j, which in enumerate((2, 5)):
                    r.append(e.dma_start(out=brow[xc:xc + 1, j * 1024:(j + 1) * 1024],
                                         in_=dr["b_mod"].ap()[i:i + 1, which * 1024:(which + 1) * 1024]))
            return r
        P.op("sp", ldb, writes=[b_brow], ndma=4, key="brow")
        for kc in range(8):
            s = it % 2
            it += 1
            P.op("sp", lambda e, i=i, kc=kc, s=s: [e.dma_start(out=wm[s][:], in_=w_mod.ap()[i, kc * 128:(kc + 1) * 128, :])],
                 writes=[b_wm[s]], ndma=1, key="wm%d" % s)

            def mmc(e, kc=kc, s=s):
                for fc in range(48):
                    r = e.matmul(pcolv[:, fc * 2:fc * 2 + 2], lhsT=wm[s][:, fc * 128:(fc + 1) * 128],
                                 rhs=sc[:, kc * 2:kc * 2 + 2], start=True, stop=True)
                return r
            P.op("pe", mmc, reads=[b_wm[s], b_sc], writes=[K.pbuf[0]])

            def mmr(e, kc=kc, s=s):
                for j, which in enumerate((2, 5)):
                    for h in range(2):
                        r = e.matmul(prow[j * 2 + h][0:2, :], lhsT=sc[:, kc * 2:kc * 2 + 2],
                                     rhs=wm[s][:, which * 1024 + h * 512: which * 1024 + (h + 1) * 512],
                                     start=True, stop=True)
                return r
            P.op("pe", mmr, reads=[b_wm[s], b_sc], writes=[K.pbuf[1], K.pbuf[2], K.pbuf[3], K.pbuf[4]])
            if kc == 0:
                P.op("dve", lambda e: e.tensor_copy(out=acc_col[:], in_=pcolv[:, 0:96]), reads=[K.pbuf[0]], writes=[b_acc_col])

                def cpr(e):
                    for q in range(4):
                        r = e.tensor_copy(out=acc_row[:, q * 512:(q + 1) * 512], in_=prow[q][0:2, :])
                    return r
                P.op("dve", cpr, reads=[K.pbuf[1], K.pbuf[2], K.pbuf[3], K.pbuf[4]], writes=[b_acc_row])
            else:
                P.op("dve", lambda e: e.tensor_tensor(out=acc_col[:], in0=pcolv[:, 0:96], in1=acc_col[:], op=ALU.add),
                     reads=[K.pbuf[0], b_acc_col], writes=[b_acc_col])

                def adr(e):
                    for q in range(4):
                        r = e.tensor_tensor(out=acc_row[:, q * 512:(q + 1) * 512], in0=prow[q][0:2, :],
                                            in1=acc_row[:, q * 512:(q + 1) * 512], op=ALU.add)
                    return r
                P.op("dve", adr, reads=[K.pbuf[1], K.pbuf[2], K.pbuf[3], K.pbuf[4], b_acc_row], writes=[b_acc_row])

        def fin_col(e, i=i):
            a3 = acc_col[:].rearrange("p (f x) -> p f x", x=2)
            m3 = C.modcol[:, i * 96:(i + 1) * 96].rearrange("p (f x) -> p f x", x=2)
            for xc in range(2):
                r = e.tensor_tensor(out=m3[:, :, xc], in0=a3[:, :, xc], in1=bmc[:], op=ALU.add)
            return r
        P.op("dve", fin_col, reads=[b_acc_col, b_bmc], writes=[C.b_modcol])
        P.op("dve", lambda e: e.tensor_tensor(out=acc_row[:], in0=acc_row[:], in1=brow[:], op=ALU.add),
             reads=[b_acc_row, b_brow], writes=[b_acc_row])
        P.op("sp", lambda e, i=i: [e.dma_start(out=dr["modrow"].ap()[i], in_=acc_row[:])], reads=[b_acc_row], ndma=1, key="modrow")

        def mk_sb(e, i=i):
            for sub in range(2):
                wsc, wsh = (1, 0) if sub == 0 else (4, 3)
                gcol = C.pcol[:, dr["pc"]["norm_g"] + (i * 2 + sub) * 8: dr["pc"]["norm_g"] + (i * 2 + sub) * 8 + 8]
                for xc in range(2):
                    o = ((i * 2 + sub) * 2 + xc) * 8
                    msc = C.modcol[:, i * 96 + wsc * 16: i * 96 + wsc * 16 + 16].rearrange("p (f x) -> p f x", x=2)[:, :, xc]
                    msh = C.modcol[:, i * 96 + wsh * 16: i * 96 + wsh * 16 + 16].rearrange("p (f x) -> p f x", x=2)[:, :, xc]
                    e.scalar_tensor_tensor(out=C.scol[:, o:o + 8], in0=msc, scalar=1.0, in1=gcol, op0=ALU.add, op1=ALU.mult)
                    r = e.tensor_copy(out=C.bcol[:, o:o + 8], in_=msh)
            return r
        P.op("dve", mk_sb, reads=[C.b_modcol, C.b_pcol], writes=[C.b_scol])


class NMT:
    def __init__(self, K, C, want32=False, scale_eng="act", ev_eng="act", pbanks=(0, 1), nslots=2):
        self.K, self.C = K, C
        self.ns = nslots
        self.xt = [K.sb("xt%d" % i, [128, D], F32) for i in range(nslots)]
        self.xs = [K.sb("xs%d" % i, [128, D], F32) for i in range(nslots)]
        self.junk = K.sb("junk", [128, D], BF16)
        self.ss = [K.sb("ss%d" % i, [128, 1], F32) for i in range(nslots)]
        self.rs = [K.sb("rs%d" % i, [128, 1], F32) for i in range(nslots)]
        self.b_xt = [Buf() for _ in range(nslots)]
        self.b_xs = [Buf() for _ in range(nslots)]
        self.b_junk = Buf()
        self.b_ss = [Buf() for _ in range(nslots)]
        self.b_rs = [Buf() for _ in range(nslots)]
        self.n = 0
        self.want32 = want32
        self.scale_eng = scale_eng
        self.ev_eng = ev_eng
        self.pbanks = pbanks

    def tile(self, src_rows, layer, sub, is_ctx, out_ap_fn, out_buf, out32_fn=None, out32_buf=None):
        K, C, P = self.K, self.C, self.K.P
        s = self.n % self.ns
        self.n += 1
        xt, xs, ss, rs = self.xt[s], self.xs[s], self.ss[s], self.rs[s]
        P.op("sp", lambda e: [e.dma_start(out=xt[:], in_=src_rows)], writes=[self.b_xt[s]], ndma=1, key="xt%d" % s)
        P.op("act", lambda e: e.activation(out=self.junk[:], in_=xt[:], func=AF.Square, accum_out=ss[:]),
             reads=[self.b_xt[s]], writes=[self.b_junk, self.b_ss[s]])
        P.op("act", lambda e: e.activation(out=ss[:], in_=ss[:], func=AF.Sqrt, scale=1.0 / D, bias=C.eps[:]),
             reads=[self.b_ss[s], C.b_eps], writes=[self.b_ss[s]])
        P.op("dve", lambda e: e.reciprocal(out=rs[:], in_=ss[:]), reads=[self.b_ss[s]], writes=[self.b_rs[s]])
        if self.scale_eng == "act":
            P.op("act", lambda e: e.activation(out=xs[:], in_=xt[:], func=AF.Copy, scale=rs[:]),
                 reads=[self.b_xt[s], self.b_rs[s]], writes=[self.b_xs[s]])
        else:
            P.op(self.scale_eng, lambda e: e.tensor_scalar(out=xs[:], in0=xt[:], scalar1=rs[:], scalar2=None, op0=ALU.mult),
                 reads=[self.b_xt[s], self.b_rs[s]], writes=[self.b_xs[s]])
        o = ((layer * 2 + sub) * 2 + (1 if is_ctx else 0)) * 8
        for half in range(2):
            bank = self.pbanks[half]
            pT = K.psb[bank][:].rearrange("p (a b) -> p a b", b=128)

            def tr(e, half=half, pT=pT):
                for j in range(4):
                    f = half * 4 + j
                    r = e.transpose(out=pT[:, j, :], in_=xs[:, f * 128:(f + 1) * 128], identity=C.ident[:])
                return r
            P.op("pe", tr, reads=[self.b_xs[s], C.b_ident], writes=[K.pbuf[bank]])

            def ev(e, half=half, pT=pT):
                for j in range(4):
                    f = half * 4 + j
                    dsts = [out_ap_fn(f)] + ([out32_fn(f)] if out32_fn is not None else [])
                    for dd in dsts:
                        if self.ev_eng == "act":
                            r = e.activation(out=dd, in_=pT[:, j, :], func=AF.Identity,
                                             scale=C.scol[:, o + f:o + f + 1], bias=C.bcol[:, o + f:o + f + 1])
                        else:
                            r = e.tensor_scalar(out=dd, in0=pT[:, j, :], scalar1=C.scol[:, o + f:o + f + 1],
                                                scalar2=C.bcol[:, o + f:o + f + 1], op0=ALU.mult, op1=ALU.add)
                return r
            wr = [out_buf] + ([out32_buf] if out32_buf is not None else [])
            P.op(self.ev_eng, ev, reads=[K.pbuf[bank], C.b_scol], writes=wr)


def groups_of(tiles, gmax):
    ng = (len(tiles) + gmax - 1) // gmax
    base, rem = divmod(len(tiles), ng)
    out, i = [], 0
    for g in range(ng):
        n = base + (1 if g < rem else 0)
        out.append(tiles[i:i + n])
        i += n
    return out


def stage_ffn(K, C, dr, layer, src, dst, tiles, moe):
    P = K.P
    j = layer // 2
    E = NE if moe else 1
    GMAX = 12
    nmt = NMT(K, C, want32=moe, scale_eng="act", ev_eng="act", pbanks=(0, 1), nslots=3)
    TMAX = GMAX * 128
    hT = K.sb("hT", [128, 8, TMAX], BF16)
    yacc = K.sb("yacc", [128, GMAX, D], F32)
    actT = K.sb("actT", [128, 4, TMAX], BF16)
    wg = [K.sb("wg%d" % i, [128, 8, 512], BF16) for i in range(2)]
    wu = [K.sb("wu%d" % i, [128, 8, 512], BF16) for i in range(2)]
    wd = [K.sb("wd%d" % i, [128, 4, D], BF16) for i in range(2)]
    sg = [K.sb("sg%d" % i, [128, 512], F32) for i in range(2)]
    b_w = [Buf(), Buf()]
    b_sg = [Buf(), Buf()]
    m5 = [K.sb("m5_%d" % i, [128, D], F32) for i in range(2)]
    b_m5 = Buf()
    xo = [K.sb("xo%d" % i, [128, D], F32) for i in range(2)]
    b_xo = [Buf(), Buf()]

    def ldm5(e):
        return [e.dma_start(out=m5[xc][:], in_=bcast_rows(dr["modrow"], (layer * 2 + xc) * 2048 + 1024, 1024)) for xc in range(2)]
    P.op("sp", ldm5, writes=[b_m5], ndma=2, key="m5")
    if moe:
        h32 = K.sb("h32", [128, 8, 128], F32)
        b_h32 = Buf()
        wr = K.sb("wr", [128, 8, NE], F32)
        b_wr = Buf()
        P.op("sp", lambda e: [e.dma_start(out=wr[:], in_=dr["moe_w_router"].ap()[j].rearrange("(k p) e -> p k e", p=128))],
             writes=[b_wr], ndma=1, key="wr")
        gates = K.sb("gates", [128, GMAX, NE], F32)
        lg = K.sb("lg", [128, NE], F32)
        eq1 = K.sb("eq1", [128, NE], F32)
        eq2 = K.sb("eq2", [128, NE], F32)
        l2 = K.sb("l2", [128, NE], F32)
        mm = K.sb("mm", [128, 4], F32)
        b_rt = Buf()
    if moe:
        wG, wU, wD = dr["moe_w_gate"].ap()[j], dr["moe_w_up"].ap()[j], dr["moe_w_down"].ap()[j]
    else:
        wG, wU, wD = dr["ffn_w_gate"].ap()[j:j + 1], dr["ffn_w_up"].ap()[j:j + 1], dr["ffn_w_down"].ap()[j:j + 1]
    nslab = DFF // 512
    wcount = [0]
    dcount = [0]

    def load_w(e_idx, s_idx):
        slot = wcount[0] % 2
        wcount[0] += 1

        def f(e, slot=slot):
            r = []
            for k in range(8):
                r.append(e.dma_start(out=wg[slot][:, k, :], in_=wG[e_idx, k * 128:(k + 1) * 128, s_idx * 512:(s_idx + 1) * 512]))
                r.append(e.dma_start(out=wu[slot][:, k, :], in_=wU[e_idx, k * 128:(k + 1) * 128, s_idx * 512:(s_idx + 1) * 512]))
            for k in range(4):
                r.append(e.dma_start(out=wd[slot][:, k, :], in_=wD[e_idx, s_idx * 512 + k * 128: s_idx * 512 + (k + 1) * 128, :]))
            return r
        P.op("poolq", f, writes=[b_w[slot]], ndma=20, key="w%d_L%d" % (slot, layer))
        return slot

    b_hT = [Buf() for _ in range(GMAX)]
    b_y = [Buf() for _ in range(GMAX)]
    b_g = [Buf() for _ in range(GMAX)]
    grps = groups_of(tiles, GMAX)

    def phase1_tile(grp, li):
        if True:
            t = grp[li]
            is_ctx = t < 2
            nmt.tile(src[t * 128:(t + 1) * 128, :], layer, 1, is_ctx,
                     lambda f, li=li: hT[:, f, li * 128:(li + 1) * 128], b_hT[li],
                     (lambda f: h32[:, f, :]) if moe else None, b_h32 if moe else None)
            if moe:
                plg = K.psb[2]

                def rmm(e):
                    for k in range(8):
                        r = e.matmul(plg[:, 0:NE], lhsT=h32[:, k, :], rhs=wr[:, k, :], start=(k == 0), stop=(k == 7))
                    return r
                P.op("pe", rmm, reads=[b_h32, b_wr], writes=[K.pbuf[2]])

                P.chain("dve", [
                    lambda e: e.tensor_copy(out=lg[:], in_=plg[:, 0:NE]),
                    lambda e: e.tensor_reduce(out=mm[:, 0:1], in_=lg[:], axis=AX.X, op=ALU.max),
                    lambda e: e.tensor_scalar(out=eq1[:], in0=lg[:], scalar1=mm[:, 0:1], scalar2=None, op0=ALU.is_equal),
                    lambda e: e.scalar_tensor_tensor(out=l2[:], in0=eq1[:], scalar=-1e30, in1=lg[:], op0=ALU.mult, op1=ALU.add),
                    lambda e: e.tensor_reduce(out=mm[:, 1:2], in_=l2[:], axis=AX.X, op=ALU.max),
                    lambda e: e.tensor_scalar(out=eq2[:], in0=l2[:], scalar1=mm[:, 1:2], scalar2=None, op0=ALU.is_equal),
                    lambda e: e.tensor_tensor(out=mm[:, 2:3], in0=mm[:, 1:2], in1=mm[:, 0:1], op=ALU.subtract),
                ], reads=[K.pbuf[2]], writes=[b_rt])
                P.op("act", lambda e: e.activation(out=mm[:, 2:3], in_=mm[:, 2:3], func=AF.Sigmoid), reads=[b_rt], writes=[b_rt])
                P.chain("dve", [
                    lambda e: e.tensor_scalar(out=mm[:, 3:4], in0=mm[:, 2:3], scalar1=-1.0, scalar2=1.0, op0=ALU.mult, op1=ALU.add),
                    lambda e, li=li: e.tensor_scalar(out=gates[:, li, :], in0=eq1[:], scalar1=mm[:, 3:4], scalar2=None, op0=ALU.mult),
                    lambda e, li=li: e.scalar_tensor_tensor(out=gates[:, li, :], in0=eq2[:], scalar=mm[:, 2:3], in1=gates[:, li, :],
                                                            op0=ALU.mult, op1=ALU.add),
                ], reads=[b_rt], writes=[b_g[li], b_rt])
    def phase3_tile(grp, li):
        t = grp[li]
        xc = 1 if t < 2 else 0
        s = nmt.n % nmt.ns
        nmt.n += 1
        xt = nmt.xt[s]
        P.op("sp", lambda e: [e.dma_start(out=xt[:], in_=src[t * 128:(t + 1) * 128, :])],
             writes=[nmt.b_xt[s]], ndma=1, key="xt%d" % s)
        so = li % 2
        P.chain("dve", [
            lambda e: e.tensor_tensor(out=xo[so][:], in0=yacc[:, li, :], in1=m5[xc][:], op=ALU.mult),
            lambda e: e.tensor_tensor(out=xo[so][:], in0=xo[so][:], in1=xt[:], op=ALU.add),
        ], reads=[b_y[li], b_m5, nmt.b_xt[s]], writes=[b_xo[so]])
        P.op("poolq", lambda e: [e.dma_start(out=dst[t * 128:(t + 1) * 128, :], in_=xo[so][:])],
             reads=[b_xo[so]], ndma=1, key="xo%d" % so)

    for li in range(len(grps[0])):
        phase1_tile(grps[0], li)
    for gi, grp in enumerate(grps):
        ntl = len(grp)
        blocks = [list(range(ntl))[i:i + 4] for i in range(0, ntl, 4)]
        b_act = [Buf() for _ in blocks]
        seq = [(e_, s_) for e_ in range(E) for s_ in range(nslab)]
        if gi == 0:
            slot_next = load_w(*seq[0])
        for qi, (e_, s_) in enumerate(seq):
            slot = slot_next
            if qi + 1 < len(seq):
                slot_next = load_w(*seq[qi + 1])
            elif gi + 1 < len(grps):
                slot_next = load_w(*seq[0])
            first = (qi == 0)

            def GU(bi, slot=slot):
                blk = blocks[bi]
                t0, n = blk[0] * 128, len(blk) * 128
                for c in range(4):
                    pg, pu = 2 + (c % 2), 4 + (c % 2)

                    def mmg(e, c=c, pg=pg, w=wg):
                        for k in range(8):
                            r = e.matmul(K.psb[pg][:, 0:n], lhsT=w[slot][:, k, c * 128:(c + 1) * 128], rhs=hT[:, k, t0:t0 + n],
                                         start=(k == 0), stop=(k == 7))
                        return r
                    P.op("pe", mmg, reads=[b_w[slot]] + [b_hT[li] for li in blk], writes=[K.pbuf[pg]])

                    def mmu(e, c=c, pu=pu):
                        for k in range(8):
                            r = e.matmul(K.psb[pu][:, 0:n], lhsT=wu[slot][:, k, c * 128:(c + 1) * 128], rhs=hT[:, k, t0:t0 + n],
                                         start=(k == 0), stop=(k == 7))
                        return r
                    P.op("pe", mmu, reads=[b_w[slot]] + [b_hT[li] for li in blk], writes=[K.pbuf[pu]])
                    P.op("act", lambda e, c=c, pg=pg: e.activation(out=sg[c % 2][:, 0:n], in_=K.psb[pg][:, 0:n], func=AF.Silu),
                         reads=[K.pbuf[pg]], writes=[b_sg[c % 2]])
                    P.op("dve", lambda e, c=c, pu=pu: e.tensor_tensor(out=actT[:, c, t0:t0 + n], in0=K.psb[pu][:, 0:n],
                                                                     in1=sg[c % 2][:, 0:n], op=ALU.mult),
                         reads=[K.pbuf[pu], b_sg[c % 2]], writes=[b_act[bi]])

            def DN(bi, slot=slot, first=first, e_=e_):
                blk = blocks[bi]
                for li in blk:
                    dcount[0] += 1
                    pd = (0, 1) if dcount[0] % 2 == 0 else (6, 7)

                    def mmd(e, li=li, pd=pd):
                        for h in range(2):
                            for c in range(4):
                                r = e.matmul(K.psb[pd[h]][:, :], lhsT=actT[:, c, li * 128:(li + 1) * 128],
                                             rhs=wd[slot][:, c, h * 512:(h + 1) * 512], start=(c == 0), stop=(c == 3))
                        return r
                    P.op("pe", mmd, reads=[b_w[slot], b_act[bi]], writes=[K.pbuf[pd[0]], K.pbuf[pd[1]]])

                    def acc(e, li=li, pd=pd):
                        for h in range(2):
                            o = yacc[:, li, h * 512:(h + 1) * 512]
                            if moe:
                                gcol = gates[:, li, e_:e_ + 1]
                                if first:
                                    r = e.tensor_scalar(out=o, in0=K.psb[pd[h]][:, :], scalar1=gcol, scalar2=None, op0=ALU.mult)
                                else:
                                    r = e.scalar_tensor_tensor(out=o, in0=K.psb[pd[h]][:, :], scalar=gcol, in1=o,
                                                               op0=ALU.mult, op1=ALU.add)
                            else:
                                if first:
                                    r = e.tensor_copy(out=o, in_=K.psb[pd[h]][:, :])
                                else:
                                    r = e.tensor_tensor(out=o, in0=K.psb[pd[h]][:, :], in1=o, op=ALU.add)
                        return r
                    P.op("dve", acc, reads=[K.pbuf[pd[0]], K.pbuf[pd[1]]] + ([b_g[li]] if moe else []) + [b_y[li]], writes=[b_y[li]])

            nb = len(blocks)
            for bi in range(nb + 1):
                if bi < nb:
                    GU(bi)
                if bi >= 1:
                    DN(bi - 1)
        nxt = grps[gi + 1] if gi + 1 < len(grps) else []
        for li in range(max(ntl, len(nxt))):
            if li < ntl:
                phase3_tile(grp, li)
            if li < len(nxt):
                phase1_tile(nxt, li)


def stage_final(K, C, dr, src, dst):
    P = K.P
    xt = [K.sb("fxt%d" % i, [128, D], F32) for i in range(2)]
    xo = [K.sb("fxo%d" % i, [128, D], F32) for i in range(2)]
    junk = K.sb("fjunk", [128, D], BF16)
    ss = [K.sb("fss%d" % i, [128, 1], F32) for i in range(2)]
    gb = K.sb("fg", [128, D], F32)
    b_xt, b_xo, b_ss = [Buf(), Buf()], [Buf(), Buf()], [Buf(), Buf()]
    b_junk, b_g = Buf(), Buf()
    P.op("sp", lambda e: [e.dma_start(out=gb[:], in_=bcast_rows(dr["prow"], dr["pr"]["final_g"] * 1024, 1024))],
         writes=[b_g], ndma=1, key="fg")
    for t in range(2, NT):
        s = t % 2
        P.op("sp", lambda e, t=t, s=s: [e.dma_start(out=xt[s][:], in_=src[t * 128:(t + 1) * 128, :])],
             writes=[b_xt[s]], ndma=1, key="fxt%d" % s)
        P.op("act", lambda e, s=s: e.activation(out=junk[:], in_=xt[s][:], func=AF.Square, accum_out=ss[s][:]),
             reads=[b_xt[s]], writes=[b_junk, b_ss[s]])
        P.op("act", lambda e, s=s: e.activation(out=ss[s][:], in_=ss[s][:], func=AF.Sqrt, scale=1.0 / D, bias=C.eps[:]),
             reads=[b_ss[s], C.b_eps], writes=[b_ss[s]])
        P.op("dve", lambda e, s=s: e.reciprocal(out=ss[s][:], in_=ss[s][:]), reads=[b_ss[s]], writes=[b_ss[s]])
        P.op("dve", lambda e, s=s: e.scalar_tensor_tensor(out=xo[s][:], in0=xt[s][:], scalar=ss[s][:], in1=gb[:],
                                                           op0=ALU.mult, op1=ALU.mult),
             reads=[b_xt[s], b_ss[s], b_g], writes=[b_xo[s]])
        P.op("poolq", lambda e, t=t, s=s: [e.dma_start(out=dst[(t - 2) * 128:(t - 1) * 128, :], in_=xo[s][:])],
             reads=[b_xo[s]], ndma=1, key="fxo%d" % s)


def col_layout(v):
    v = np.asarray(v, np.float32).reshape(-1)
    return np.ascontiguousarray(v.reshape(-1, 128).T)


def pack_params(inp):
    pc, cols = {}, []
    n = 0

    def add(name, arr):
        nonlocal n
        a = col_layout(arr)
        pc[name] = n
        cols.append(a)
        n += a.shape[1]
    add("norm_g", np.concatenate([col_layout(inp["norm_g"][i, s_]) for i in range(4) for s_ in range(2)], axis=1).T.reshape(-1)
        if False else np.stack([inp["norm_g"][i, s_] for i in range(4) for s_ in range(2)]).reshape(-1))
    add("conv_b_in", inp["conv_b_in"].reshape(-1))
    add("conv_b_dw", inp["conv_b_dw"].reshape(-1))
    add("conv_ln_g", inp["conv_ln_g"].reshape(-1))
    add("conv_ln_b", inp["conv_ln_b"].reshape(-1))
    add("conv_w_dw", inp["conv_w_dw"].reshape(-1))
    add("attn_subln_g", inp["attn_subln_g"].reshape(-1))
    pcol = np.ascontiguousarray(np.concatenate(cols, axis=1))
    pr = {"conv_b_out": 0, "fnet_b": 2, "final_g": 3}
    prow = np.ascontiguousarray(np.concatenate([inp["conv_b_out"].reshape(2, D), inp["fnet_b"].reshape(1, D),
                                                inp["final_g"].reshape(1, D)], axis=0).astype(np.float32))
    return pcol, pc, prow, pr


BIG = ["w_mod", "b_mod", "conv_w_in", "conv_w_out", "fnet_w", "attn_w_qkv", "attn_w_o", "attn_lambda",
       "ffn_w_gate", "ffn_w_up", "ffn_w_down", "moe_w_router", "moe_w_gate", "moe_w_up", "moe_w_down"]


def needed_inputs(plan):
    need = {"ident", "pcol", "prow", "ccol", "xin"}
    for st in plan:
        if st == "mod":
            need |= {"w_mod", "b_mod", "bmodcol"}
        elif st in ("ffn0", "ffn2"):
            need |= {"ffn_w_gate", "ffn_w_up", "ffn_w_down"}
        elif st in ("ffn1", "ffn3"):
            need |= {"moe_w_router", "moe_w_gate", "moe_w_up", "moe_w_down"}
        elif st in ("mix0", "mix3"):
            need |= {"conv_w_in", "conv_w_out"}
        elif st == "mix1":
            need |= {"fnet_w", "dft128", "dftx", "dftc"}
        elif st == "mix2":
            need |= {"attn_w_qkv", "attn_w_o", "attn_lambda", "rope"}
    return need


def host_shared(inp, need):
    sh = {}
    pcol, pc, prow, pr = pack_params(inp)
    sh["pcol"], sh["prow"] = pcol, prow
    sh["ident"] = np.eye(128, dtype=np.float32)
    for k in BIG:
        if k in need:
            sh[k] = np.ascontiguousarray(np.asarray(inp[k], np.float32))
    if "dft128" in need:
        sh["dft128"], sh["dftx"], sh["dftc"] = dft_tables()
    if "rope" in need:
        sh["rope"] = rope_table()
    if "bmodcol" in need:
        sh["bmodcol"] = np.ascontiguousarray(np.asarray(inp["b_mod"], np.float32).reshape(4, 48, 128).transpose(0, 2, 1))
    return sh, pc, pr


def host_core(inp, b):
    m = {}
    m["xin"] = np.ascontiguousarray(np.concatenate([inp["ctx"][b], inp["x"][b]], axis=0).astype(np.float32))
    cc = np.zeros((128, 16), np.float32)
    cc[:, 0::2] = col_layout(inp["c"][b])
    cc[:, 1::2] = col_layout(inp["c_ctx"])
    m["ccol"] = cc
    return m


def build(plan, shapes, pc, pr, modcol_in=False):
    K = KB()
    need = needed_inputs(plan)
    dr = {"pc": pc, "pr": pr}
    for name in sorted(need):
        dr[name] = K.din(name, shapes[name][0], BF16 if shapes[name][1] == "bf16" else F32)
    dr["pcol_n"] = shapes["pcol"][0][1]
    dr["modrow"] = K.dscr("modrow", [4, 2, 2048])
    xs = K.dscr("xs", [NTOK, D])
    data = [st for st in plan if st != "mod"]
    fin = len(data) > 0 and data[-1] == "final"
    if fin:
        out = K.dout("y", [NX, D])
    else:
        out = K.dout("xout", [NTOK, D])
    K.init_psum()
    C = setup_common(K, dr)
    K.persist()
    for st in plan:
        if st == "mod":
            stage_mod(K, C, dr)
            K.stage_reset()
            continue
        i = data.index(st)
        src = dr["xin"].ap() if i == 0 else xs.ap()
        dst = out.ap() if i == len(data) - 1 else xs.ap()
        if st.startswith("ffn"):
            layer = int(st[3])
            tiles = list(range(NT)) if layer < 3 else list(range(2, NT))
            stage_ffn(K, C, dr, layer, src, dst, tiles, moe=(layer % 2 == 1))
        elif st.startswith("mix"):
            layer = int(st[3])
            MIXERS[layer % 3](K, C, dr, layer, src, dst)
        elif st == "final":
            stage_final(K, C, dr, src, dst)
        K.stage_reset()
    dma_keys = sorted({o.key for o in K.P.ops if o.ndma})
    K.P.emit(K.nc, final_wait_keys=dma_keys)
    return K


MIXERS = {}


def run_plan(inp, plan, ncores=8, trace=False):
    need = needed_inputs(plan)
    sh, pc, pr = host_shared(inp, need)
    in_maps = []
    for b in range(ncores):
        m = dict(sh)
        m.update(host_core(inp, b))
        in_maps.append({k: v for k, v in m.items() if k in need})
    shapes = {k: (v.shape, v.dtype.type if v.dtype != ml_dtypes.bfloat16 else "bf16") for k, v in in_maps[0].items()}
    K = build(plan, shapes, pc, pr)
    res = run_bass_kernel_spmd(K.nc, in_maps, core_ids=list(range(ncores)), trace=trace)
    return res, K


def stage_conv(K, C, dr, layer, src, dst):
    P = K.P
    j = layer // 3
    pc = dr["pc"]
    seqs = []
    if layer < 3:
        seqs.append((0, 2, True))
    seqs.append((2, 32, False))
    uT = {}
    for (t0, ntl, is_ctx) in seqs:
        L = ntl * 128
        uT[t0] = K.sb("uT%d" % t0, [128, 8, L + 30], BF16)
    mark = K.sb_off
    b_pad = Buf()

    def zero_pads(e):
        for (t0, ntl, is_ctx) in seqs:
            L = ntl * 128
            e.memset(uT[t0][:, :, 0:15], 0.0)
            r = e.memset(uT[t0][:, :, 15 + L:30 + L], 0.0)
        return r
    P.op("dve", zero_pads, writes=[b_pad])
    nmt = NMT(K, C, scale_eng="act", ev_eng="act", pbanks=(0, 1), nslots=3)
    win = K.sb("win", [128, 8, 2048], BF16)
    b_win = Buf()

    def ld_win(e):
        return [e.dma_start(out=win[:, k, :], in_=dr["conv_w_in"].ap()[j, k * 128:(k + 1) * 128, :]) for k in range(8)]
    P.op("poolq", ld_win, writes=[b_win], ndma=8, key="win")
    hTb = [K.sb("hTb%d" % i, [128, 8, 512], BF16) for i in range(2)]
    b_hTb = [Buf(), Buf()]
    sig = [K.sb("sig%d" % i, [128, 512], F32) for i in range(2)]
    b_sig = [Buf(), Buf()]
    b_u = Buf()
    bi_in = pc["conv_b_in"] + j * 16
    nblk = 0
    for (t0, ntl, is_ctx) in seqs:
        for b0 in range(0, ntl, 4):
            bt = list(range(b0, min(b0 + 4, ntl)))
            n = len(bt) * 128
            s = nblk % 2
            nblk += 1
            for li, t in enumerate(bt):
                nmt.tile(src[(t0 + t) * 128:(t0 + t + 1) * 128, :], layer, 0, is_ctx,
                         lambda f, li=li, s=s: hTb[s][:, f, li * 128:(li + 1) * 128], b_hTb[s])
            for c in range(8):
                pa, pg = 2 + (c % 2), 4 + (c % 2)

                def mma(e, c=c, pa=pa, s=s, n=n):
                    for k in range(8):
                        r = e.matmul(K.psb[pa][:, 0:n], lhsT=win[:, k, c * 128:(c + 1) * 128], rhs=hTb[s][:, k, 0:n],
                                     start=(k == 0), stop=(k == 7))
                    return r
                P.op("pe", mma, reads=[b_win, b_hTb[s]], writes=[K.pbuf[pa]])

                def mmg(e, c=c, pg=pg, s=s, n=n):
                    for k in range(8):
                        r = e.matmul(K.psb[pg][:, 0:n], lhsT=win[:, k, 1024 + c * 128:1024 + (c + 1) * 128], rhs=hTb[s][:, k, 0:n],
                                     start=(k == 0), stop=(k == 7))
                    return r
                P.op("pe", mmg, reads=[b_win, b_hTb[s]], writes=[K.pbuf[pg]])
                P.op("act", lambda e, c=c, pg=pg, n=n: e.activation(out=sig[c % 2][:, 0:n], in_=K.psb[pg][:, 0:n], func=AF.Sigmoid,
                                                                  bias=C.pcol[:, bi_in + 8 + c: bi_in + 9 + c]),
                     reads=[K.pbuf[pg], C.b_pcol], writes=[b_sig[c % 2]])
                P.op("dve", lambda e, c=c, pa=pa, n=n, t0=t0, b0=b0: e.scalar_tensor_tensor(
                    out=uT[t0][:, c, 15 + b0 * 128: 15 + b0 * 128 + n], in0=K.psb[pa][:, 0:n],
                    scalar=C.pcol[:, bi_in + c: bi_in + c + 1], in1=sig[c % 2][:, 0:n], op0=ALU.add, op1=ALU.mult),
                     reads=[K.pbuf[pa], b_sig[c % 2], C.b_pcol, b_pad], writes=[b_u])
    P.barrier()
    K.sb_off = mark
    NB = 256
    dg = K.sb("dg", [128, 8, 31, 128], BF16)
    b_dg = Buf()
    wdw = pc["conv_w_dw"] + j * 31 * 8

    def mk_dg(e):
        for c in range(8):
            for tap in range(31):
                r = e.tensor_scalar(out=dg[:, c, tap, :], in0=C.ident[:], scalar1=C.pcol[:, wdw + tap * 8 + c: wdw + tap * 8 + c + 1],
                                    scalar2=None, op0=ALU.mult)
        return r
    P.op("dve", mk_dg, reads=[C.b_ident, C.b_pcol], writes=[b_dg])
    wout = K.sb("wout", [128, 8, D], BF16)
    b_wout = Buf()
    P.op("poolq", lambda e: [e.dma_start(out=wout[:, k, :], in_=dr["conv_w_out"].ap()[j, k * 128:(k + 1) * 128, :]) for k in range(8)],
         writes=[b_wout], ndma=8, key="wout")
    eps5 = K.sb("eps5", [128, 1], F32)
    b_eps5 = Buf()
    P.op("dve", lambda e: e.memset(eps5[:], 1e-5), writes=[b_eps5])
    v32 = K.sb("v32", [128, 8, NB], F32)
    b_v = [Buf() for _ in range(8)]
    sq = [K.sb("sq%d" % i, [128, NB], F32) for i in range(2)]
    b_sq = [Buf(), Buf()]
    z = K.sb("z", [128, 8, NB], BF16)
    b_z = Buf()
    mean = K.sb("mean", [128, NB], F32)
    msq = K.sb("msq", [128, NB], F32)
    rstd = K.sb("rstd", [128, NB], F32)
    b_st = Buf()
    t1 = [K.sb("t1_%d" % i, [128, NB], F32) for i in range(2)]
    b_t1 = [Buf(), Buf()]
    xt = [K.sb("cxt%d" % i, [128, D], F32) for i in range(2)]
    xo = [K.sb("cxo%d" % i, [128, D], F32) for i in range(2)]
    b_xt, b_xo = [Buf(), Buf()], [Buf(), Buf()]
    m2 = [K.sb("m2_%d" % i, [128, D], F32) for i in range(2)]
    mb = [K.sb("mb_%d" % i, [128, D], F32) for i in range(2)]
    b_m2 = Buf()

    def ldm2(e):
        r = [e.dma_start(out=m2[xc][:], in_=bcast_rows(dr["modrow"], (layer * 2 + xc) * 2048, 1024)) for xc in range(2)]
        for xc in range(2):
            r.append(e.dma_start(out=mb[xc][:], in_=bcast_rows(dr["prow"], (dr["pr"]["conv_b_out"] + j) * 1024, 1024)))
        return r
    P.op("sp", ldm2, writes=[b_m2], ndma=4, key="m2")

    def mkmb(e):
        for xc in range(2):
            r = e.tensor_tensor(out=mb[xc][:], in0=m2[xc][:], in1=mb[xc][:], op=ALU.mult)
        return r
    P.op("dve", mkmb, reads=[b_m2], writes=[b_m2])
    cb_dw = pc["conv_b_dw"] + j * 8
    cg = pc["conv_ln_g"] + j * 8
    cbb = pc["conv_ln_b"] + j * 8
    ntile_out = 0
    for (t0, ntl, is_ctx) in seqs:
        L = ntl * 128
        xc = 1 if is_ctx else 0
        for p0 in range(0, L, NB):
            def conv(c, p0=p0, t0=t0):
                def f(e):
                    for tap in range(31):
                        r = e.matmul(K.psb[c % 2][:, 0:NB], lhsT=dg[:, c, tap, :], rhs=uT[t0][:, c, p0 + tap: p0 + tap + NB],
                                     start=(tap == 0), stop=(tap == 30))
                    return r
                P.op("pe", f, reads=[b_dg], writes=[K.pbuf[c % 2]])
                P.op("act", lambda e: e.activation(out=v32[:, c, :], in_=K.psb[c % 2][:, 0:NB], func=AF.Identity,
                                                   bias=C.pcol[:, cb_dw + c: cb_dw + c + 1]),
                     reads=[K.pbuf[c % 2], C.b_pcol], writes=[b_v[c]])
                P.op("act", lambda e: e.activation(out=sq[c % 2][:], in_=v32[:, c, :], func=AF.Square),
                     reads=[b_v[c]], writes=[b_sq[c % 2]])

            def stats(c):
                def f(e):
                    e.matmul(K.psb[2][:, 0:NB], lhsT=C.ones[:], rhs=v32[:, c, :], start=(c == 0), stop=(c == 7))
                    return e.matmul(K.psb[3][:, 0:NB], lhsT=C.ones[:], rhs=sq[c % 2][:], start=(c == 0), stop=(c == 7))
                P.op("pe", f, reads=[b_v[c], b_sq[c % 2], C.b_ones], writes=[K.pbuf[2], K.pbuf[3]])
            for c in range(9):
                if c < 8:
                    conv(c)
                if c >= 1:
                    stats(c - 1)

            P.chain("dve", [
                lambda e: e.tensor_scalar(out=mean[:], in0=K.psb[2][:, 0:NB], scalar1=1.0 / D, scalar2=None, op0=ALU.mult),
                lambda e: e.tensor_tensor(out=msq[:], in0=mean[:], in1=mean[:], op=ALU.mult),
                lambda e: e.scalar_tensor_tensor(out=rstd[:], in0=K.psb[3][:, 0:NB], scalar=1.0 / D, in1=msq[:],
                                                 op0=ALU.mult, op1=ALU.subtract),
            ], reads=[K.pbuf[2], K.pbuf[3]], writes=[b_st])
            P.op("act", lambda e: e.activation(out=rstd[:], in_=rstd[:], func=AF.Sqrt, bias=eps5[:]),
                 reads=[b_st, b_eps5], writes=[b_st])
            P.op("dve", lambda e: e.reciprocal(out=rstd[:], in_=rstd[:]), reads=[b_st], writes=[b_st])
            for c in range(8):
                P.chain("dve", [
                    lambda e, c=c: e.tensor_tensor(out=t1[c % 2][:], in0=v32[:, c, :], in1=mean[:], op=ALU.subtract),
                    lambda e, c=c: e.tensor_tensor(out=t1[c % 2][:], in0=t1[c % 2][:], in1=rstd[:], op=ALU.mult),
                ], reads=[b_v[c], b_st], writes=[b_t1[c % 2]])
                P.op("act", lambda e, c=c: e.activation(out=z[:, c, :], in_=t1[c % 2][:], func=AF.Silu,
                                                        scale=C.pcol[:, cg + c: cg + c + 1], bias=C.pcol[:, cbb + c: cbb + c + 1]),
                     reads=[b_t1[c % 2], C.b_pcol], writes=[b_z])
            for q in range(NB // 128):
                t = t0 + (p0 // 128) + q
                s = ntile_out % 2
                ntile_out += 1
                pb = 4 + 2 * s

                def mmo(e, q=q, pb=pb):
                    for h in range(2):
                        for c in range(8):
                            r = e.matmul(K.psb[pb + h][:, :], lhsT=z[:, c, q * 128:(q + 1) * 128], rhs=wout[:, c, h * 512:(h + 1) * 512],
                                         start=(c == 0), stop=(c == 7))
                    return r
                P.op("pe", mmo, reads=[b_z, b_wout], writes=[K.pbuf[pb], K.pbuf[pb + 1]])
                P.op("sp", lambda e, t=t, s=s: [e.dma_start(out=xt[s][:], in_=src[t * 128:(t + 1) * 128, :])],
                     writes=[b_xt[s]], ndma=1, key="cxt%d" % s)

                def res0(e, s=s, pb=pb, xc=xc):
                    for h in range(2):
                        r = e.tensor_tensor(out=xo[s][:, h * 512:(h + 1) * 512], in0=K.psb[pb + h][:, :],
                                            in1=m2[xc][:, h * 512:(h + 1) * 512], op=ALU.mult)
                    return r
                P.chain("dve", [
                    res0,
                    lambda e, s=s, xc=xc: e.tensor_tensor(out=xo[s][:], in0=xo[s][:], in1=mb[xc][:], op=ALU.add),
                    lambda e, s=s: e.tensor_tensor(out=xo[s][:], in0=xo[s][:], in1=xt[s][:], op=ALU.add),
                ], reads=[K.pbuf[pb], K.pbuf[pb + 1], b_m2, b_xt[s]], writes=[b_xo[s]])
                P.op("poolq", lambda e, t=t, s=s: [e.dma_start(out=dst[t * 128:(t + 1) * 128, :], in_=xo[s][:])],
                     reads=[b_xo[s]], ndma=1, key="cxo%d" % s)


MIXERS[0] = stage_conv


def dft_tables():
    d = np.arange(128)
    ang = 2 * np.pi * ((d[:, None] * d[None, :]) % 128) / 128.0
    t128 = np.concatenate([np.cos(ang), np.sin(ang)], axis=1) / np.sqrt(128.0)

    def seq_table(N):
        A = N // 128
        p = np.arange(128)
        a = np.arange(A)
        n = (a[None, :] * 128 + p[:, None])
        k = np.arange(N).reshape(A, 128)
        prod = (n[None, :, :, None].astype(np.int64) * k[:, None, None, :].astype(np.int64)) % N
        ang = prod.astype(np.float64) * (2 * np.pi / N)
        s = 1.0 / np.sqrt(N)
        tb = np.stack([np.cos(ang) * s, -np.sin(ang) * s], axis=2)
        return np.ascontiguousarray(tb.astype(np.float32).astype(ml_dtypes.bfloat16))
    return t128.astype(np.float32), seq_table(NX), seq_table(NCTX)


def stage_fnet(K, C, dr, layer, src, dst):
    P = K.P
    Hc = K.sb("Hc", [128, 32, D], BF16)
    Hs = K.sb("Hs", [128, 32, D], BF16)
    wf = K.sb("wf", [128, 8, D], BF16)
    t128 = K.sb("t128", [128, 256], BF16)
    m2s = K.sb("m2s", [128, D], F32)
    onesb = K.sb("onesb", [1, 128], BF16)
    fbb = K.sb("fbb", [1, D], BF16)
    b_wf, b_t128, b_m2s, b_ob = Buf(), Buf(), Buf(), Buf()
    P.op("poolq", lambda e: [e.dma_start(out=wf[:, k, :], in_=dr["fnet_w"].ap()[0, k * 128:(k + 1) * 128, :]) for k in range(8)],
         writes=[b_wf], ndma=8, key="wf")
    P.op("poolq", lambda e: [e.dma_start(out=t128[:], in_=dr["dft128"].ap()),
                             e.dma_start(out=fbb[:], in_=dr["prow"].ap()[dr["pr"]["fnet_b"]:dr["pr"]["fnet_b"] + 1, :])],
         writes=[b_t128, b_ob], ndma=2, key="t128")
    P.op("dve", lambda e: e.memset(onesb[:], 1.0), writes=[b_ob])
    mark = K.sb_off

    def do_seq(t0, ntl, is_ctx):
        xc = 1 if is_ctx else 0
        A = ntl
        K.sb_off = mark
        nmt = NMT(K, C, scale_eng="act", ev_eng="act", pbanks=(0, 1), nslots=3)
        hT = [K.sb("fhT%d" % i, [128, 8, 128], BF16) for i in range(2)]
        b_hT = [Buf(), Buf()]
        b_H = [Buf() for _ in range(A)]
        for a in range(A):
            s = a % 2
            nmt.tile(src[(t0 + a) * 128:(t0 + a + 1) * 128, :], layer, 0, is_ctx,
                     lambda f, s=s: hT[s][:, f, :], b_hT[s])

            def mmh(e, s=s):
                for g in range(8):
                    bank = 2 + g // 2
                    r = e.matmul(K.psb[bank][:, (g % 2) * 256:(g % 2) * 256 + 256], lhsT=hT[s][:, g, :], rhs=t128[:],
                                 start=True, stop=True)
                return r
            P.op("pe", mmh, reads=[b_hT[s], b_t128], writes=[K.pbuf[2], K.pbuf[3], K.pbuf[4], K.pbuf[5]])

            def evh(e, a=a):
                for b in range(4):
                    v = K.psb[2 + b][:].rearrange("p (g c q) -> p g c q", g=2, c=2)
                    e.tensor_copy(out=Hc[:, a, b * 256:(b + 1) * 256].rearrange("p (g q) -> p g q", g=2), in_=v[:, :, 0, :])
                    r = e.tensor_copy(out=Hs[:, a, b * 256:(b + 1) * 256].rearrange("p (g q) -> p g q", g=2), in_=v[:, :, 1, :])
                return r
            P.op("dve", evh, reads=[K.pbuf[2], K.pbuf[3], K.pbuf[4], K.pbuf[5]], writes=[b_H[a]])
        P.barrier()
        K.sb_off = mark
        tb = [K.sb("tb%d" % i, [128, 2, A, 128], BF16) for i in range(2)]
        b_tb = [Buf(), Buf()]
        fsb = K.sb("fsb", [128, D], F32)
        fT = [K.sb("fT%d" % i, [128, 8, 128], BF16) for i in range(2)]
        xt0 = K.sb("nxt0", [128, D], F32)
        xt = [xt0, xt0]
        xo = K.sb("nxo", [128, D], F32)
        bx0 = Buf()
        b_fsb, b_fT, b_xt, b_xo = Buf(), [Buf(), Buf()], [bx0, bx0], Buf()
        P.op("sp", lambda e, xc=xc: [e.dma_start(out=m2s[:], in_=bcast_rows(dr["modrow"], (layer * 2 + xc) * 2048, 1024))],
             writes=[b_m2s], ndma=1, key="m2s")
        tbl = dr["dftc"] if is_ctx else dr["dftx"]

        def ld_tb(kc, s):
            P.op("sp", lambda e: [e.dma_start(out=tb[s][:], in_=tbl.ap()[kc])], writes=[b_tb[s]], ndma=1, key="tb%d" % s)
        ld_tb(0, 0)
        for kc in range(A):
            s = kc % 2
            if kc + 1 < A:
                ld_tb(kc + 1, (kc + 1) % 2)
            fb = (0, 1) if s == 0 else (6, 7)

            def mmf(e, s=s, fb=fb):
                for h in range(2):
                    i = 0
                    for a in range(A):
                        for cs, Hx in ((0, Hc), (1, Hs)):
                            r = e.matmul(K.psb[fb[h]][:, :], lhsT=tb[s][:, cs, a, :], rhs=Hx[:, a, h * 512:(h + 1) * 512],
                                         start=(i == 0), stop=(i == 2 * A - 1))
                            i += 1
                return r
            P.op("pe", mmf, reads=[b_tb[s]] + b_H, writes=[K.pbuf[fb[0]], K.pbuf[fb[1]]])

            def evf(e, fb=fb):
                for h in range(2):
                    r = e.activation(out=fsb[:, h * 512:(h + 1) * 512], in_=K.psb[fb[h]][:, :], func=AF.Copy)
                return r
            P.op("act", evf, reads=[K.pbuf[fb[0]], K.pbuf[fb[1]]], writes=[b_fsb])
            for half in range(2):
                pT = K.psb[2 + half][:].rearrange("p (a b) -> p a b", b=128)

                def tr(e, half=half, pT=pT):
                    for q in range(4):
                        f = half * 4 + q
                        r = e.transpose(out=pT[:, q, :], in_=fsb[:, f * 128:(f + 1) * 128], identity=C.ident[:])
                    return r
                P.op("pe", tr, reads=[b_fsb, C.b_ident], writes=[K.pbuf[2 + half]])
                P.op("dve", lambda e, half=half, pT=pT, s=s: e.tensor_copy(out=fT[s][:, half * 4:(half + 1) * 4, :], in_=pT[:, :, :]),
                     reads=[K.pbuf[2 + half]], writes=[b_fT[s]])

            def mmo(e, s=s):
                for h in range(2):
                    for c in range(8):
                        e.matmul(K.psb[4 + h][:, :], lhsT=fT[s][:, c, :], rhs=wf[:, c, h * 512:(h + 1) * 512],
                                 start=(c == 0), stop=False)
                    r = e.matmul(K.psb[4 + h][:, :], lhsT=onesb[0:1, :], rhs=fbb[0:1, h * 512:(h + 1) * 512], start=False, stop=True)
                return r
            P.op("pe", mmo, reads=[b_fT[s], b_wf, b_ob], writes=[K.pbuf[4], K.pbuf[5]])
            t = t0 + kc
            P.op("sp", lambda e, t=t, s=s: [e.dma_start(out=xt[s][:], in_=src[t * 128:(t + 1) * 128, :])],
                 writes=[b_xt[s]], ndma=1, key="nxt%d" % s)

            def r0(e):
                for h in range(2):
                    r = e.tensor_tensor(out=xo[:, h * 512:(h + 1) * 512], in0=K.psb[4 + h][:, :], in1=m2s[:, h * 512:(h + 1) * 512], op=ALU.mult)
                return r
            P.chain("dve", [r0, lambda e, s=s: e.tensor_tensor(out=xo[:], in0=xo[:], in1=xt[s][:], op=ALU.add)],
                    reads=[K.pbuf[4], K.pbuf[5], b_m2s, b_xt[s]], writes=[b_xo])
            import os
            if os.environ.get("FNET_DEBUG") and is_ctx:
                dbg = {"1": fsb, "2": m2s, "3": xt0}[os.environ["FNET_DEBUG"]]
                P.op("sp", lambda e, t=t, dbg=dbg: [e.dma_start(out=dst[t * 128:(t + 1) * 128, :], in_=dbg[:])], reads=[b_xo, b_fsb, b_m2s, bx0], ndma=1, key="nxo")
            else:
                P.op("poolq", lambda e, t=t: [e.dma_start(out=dst[t * 128:(t + 1) * 128, :], in_=xo[:])], reads=[b_xo], ndma=1, key="nxo")
        P.barrier()
    do_seq(0, 2, True)
    do_seq(2, 32, False)


MIXERS[1] = stage_fnet


def rope_table():
    rows = NX // 64
    row = np.repeat(np.arange(rows, dtype=np.float32), 64)
    col = np.tile(np.arange(64, dtype=np.float32), rows)
    inv = (1.0 / (10000.0 ** (np.arange(16, dtype=np.float32) * 2.0 / 32))).astype(np.float32)
    ang = np.stack([row[:, None] * inv, col[:, None] * inv], axis=1)
    ang = np.stack([ang, ang], axis=2).reshape(NX, 64).astype(np.float32)
    cos = np.cos(ang).astype(np.float32)
    sin = np.sin(ang).astype(np.float32).reshape(NX, 2, 2, 16).copy()
    sin[:, :, 0, :] *= -1.0
    return np.ascontiguousarray(np.concatenate([cos, sin.reshape(NX, 64)], axis=1).astype(np.float32))


def sb_bcast(t, off, pstep, dims):
    return bass.AP(t, off, [[pstep, 128]] + dims)


def stage_attn(K, C, dr, layer, src, dst):
    import math
    P = K.P
    lam_init = 0.8 - 0.6 * math.exp(-0.3 * layer)
    QT = K.dscr("QTs", [NT, 128, 8, 128], BF16)
    KT = K.dscr("KTs", [NT, 128, 8, 128], BF16)
    VS = K.dscr("VSs", [NT, 128, D], BF16)
    lv = K.sb("lv", [128, 256], F32)
    lt = K.sb("lt", [128, 128], F32)
    ls = K.sb("ls", [128, 4], F32)
    gs = K.sb("gs", [128, 1], F32)
    b_lv, b_lam = Buf(), Buf()
    P.op("sp", lambda e: [e.dma_start(out=lv[:], in_=bcast_rows(dr["attn_lambda"], 0, 256))], writes=[b_lv], ndma=1, key="lv")
    P.chain("dve", [
        lambda e: e.tensor_tensor(out=lt[:, 0:64], in0=lv[:, 0:64], in1=lv[:, 64:128], op=ALU.mult),
        lambda e: e.tensor_tensor(out=lt[:, 64:128], in0=lv[:, 128:192], in1=lv[:, 192:256], op=ALU.mult),
        lambda e: e.tensor_reduce(out=ls[:, 0:1], in_=lt[:, 0:64], axis=AX.X, op=ALU.add),
        lambda e: e.tensor_reduce(out=ls[:, 1:2], in_=lt[:, 64:128], axis=AX.X, op=ALU.add),
    ], reads=[b_lv], writes=[b_lam])
    P.op("act", lambda e: e.activation(out=ls[:, 0:2], in_=ls[:, 0:2], func=AF.Exp), reads=[b_lam], writes=[b_lam])
    sg_col = dr["pc"]["attn_subln_g"]
    P.chain("dve", [
        lambda e: e.tensor_tensor(out=ls[:, 2:3], in0=ls[:, 1:2], in1=ls[:, 0:1], op=ALU.subtract),
        lambda e: e.tensor_scalar(out=ls[:, 3:4], in0=ls[:, 2:3], scalar1=-lam_init, scalar2=None, op0=ALU.add),
        lambda e: e.tensor_scalar(out=gs[:], in0=C.pcol[:, sg_col:sg_col + 1], scalar1=1.0 - lam_init, scalar2=None, op0=ALU.mult),
    ], reads=[b_lam, C.b_pcol], writes=[b_lam])
    nlam = ls[:, 3:4]
    AT = K.sb("AT", [128, 8, NTOK], BF16)
    mark = K.sb_off
    nmt = NMT(K, C, scale_eng="act", ev_eng="act", pbanks=(0, 1))
    hT = [K.sb("ahT%d" % i, [128, 8, 128], BF16) for i in range(2)]
    b_hT = [Buf(), Buf()]
    wq = K.sb("wqkv", [128, 8, 3072], BF16)
    b_wq = Buf()
    P.op("poolq", lambda e: [e.dma_start(out=wq[:, k, :], in_=dr["attn_w_qkv"].ap()[0, k * 128:(k + 1) * 128, :]) for k in range(8)],
         writes=[b_wq], ndma=8, key="wqkv")
    qk_ = [K.sb("qk%d" % i, [128, 2048], F32) for i in range(2)]
    ra_ = [K.sb("ra%d" % i, [128, 2048], F32) for i in range(2)]
    rb = K.sb("rb", [128, 2048], F32)
    cs = [K.sb("cs%d" % i, [128, 128], F32) for i in range(2)]
    vb = [K.sb("vb%d" % i, [128, D], BF16) for i in range(2)]
    qT = [K.sb("qT%d" % i, [128, 16, 128], BF16) for i in range(2)]
    b_qk_, b_ra_, b_cs, b_vb, b_qT = [Buf(), Buf()], [Buf(), Buf()], [Buf(), Buf()], [Buf(), Buf()], [Buf(), Buf()]
    b_rb = Buf()

    def a_nmt(t):
        s = t % 2
        nmt.tile(src[t * 128:(t + 1) * 128, :], layer, 0, t < 2, lambda f, s=s: hT[s][:, f, :], b_hT[s])
    a_nmt(0)
    for t in range(NT):
        s = t % 2
        is_ctx = t < 2
        qk, ra, b_qk, b_ra = qk_[s], ra_[s], b_qk_[s], b_ra_[s]
        if t + 1 < NT:
            a_nmt(t + 1)
        def mmq(nb, bank, s=s):
            def mm(e):
                for k in range(8):
                    r = e.matmul(K.psb[bank][:, :], lhsT=hT[s][:, k, :], rhs=wq[:, k, nb * 512:(nb + 1) * 512], start=(k == 0), stop=(k == 7))
                return r
            P.op("pe", mm, reads=[b_hT[s], b_wq], writes=[K.pbuf[bank]])
        for nb in range(3):
            mmq(nb, 2 + nb)

        def evqkA(e, qk=qk):
            for nb in range(3):
                r = e.activation(out=qk[:, nb * 512:(nb + 1) * 512], in_=K.psb[2 + nb][:, :], func=AF.Copy)
            return r
        P.op("act", evqkA, reads=[K.pbuf[2], K.pbuf[3], K.pbuf[4]], writes=[b_qk])
        for nb in range(3, 6):
            mmq(nb, 2 + nb - 3)
        P.op("act", lambda e, qk=qk: e.activation(out=qk[:, 1536:2048], in_=K.psb[2][:, :], func=AF.Copy),
             reads=[K.pbuf[2]], writes=[b_qk])

        def evv(e, s=s):
            for nb in range(2):
                r = e.tensor_copy(out=vb[s][:, nb * 512:(nb + 1) * 512], in_=K.psb[3 + nb][:, :])
            return r
        P.op("dve", evv, reads=[K.pbuf[3], K.pbuf[4]], writes=[b_vb[s]])
        P.op("poolq", lambda e, t=t, s=s: [e.dma_start(out=VS.ap()[t], in_=vb[s][:])], reads=[b_vb[s]], ndma=1, key="vb%d" % s)
        if not is_ctx:
            P.op("sp", lambda e, t=t, s=s: [e.dma_start(out=cs[s][:], in_=dr["rope"].ap()[(t - 2) * 128:(t - 1) * 128, :])],
                 writes=[b_cs[s]], ndma=1, key="cs%d" % s)
            qk3 = qk[:].rearrange("p (g d) -> p g d", d=64)
            ra3 = ra[:].rearrange("p (g d) -> p g d", d=64)
            qk4 = qk[:].rearrange("p (g a h f) -> p g a h f", a=2, h=2, f=16)
            rb4 = rb[:].rearrange("p (g a h f) -> p g a h f", a=2, h=2, f=16)
            cosb = sb_bcast(cs[s], 0, 128, [[0, 32], [1, 64]])
            P.chain("dve", [
                lambda e, cosb=cosb, ra3=ra3, qk3=qk3: e.tensor_tensor(out=ra3, in0=qk3, in1=cosb, op=ALU.mult),
                lambda e, s=s, qk4=qk4: [e.tensor_tensor(out=rb4[:, :, ax, 0, :], in0=qk4[:, :, ax, 1, :],
                                                in1=sb_bcast(cs[s], 64 + ax * 32, 128, [[0, 32], [1, 16]]), op=ALU.mult) for ax in range(2)][-1],
                lambda e, s=s, qk4=qk4: [e.tensor_tensor(out=rb4[:, :, ax, 1, :], in0=qk4[:, :, ax, 0, :],
                                                in1=sb_bcast(cs[s], 64 + ax * 32 + 16, 128, [[0, 32], [1, 16]]), op=ALU.mult) for ax in range(2)][-1],
                lambda e, ra=ra: e.tensor_tensor(out=ra[:], in0=ra[:], in1=rb[:], op=ALU.add),
            ], reads=[b_qk, b_cs[s]], writes=[b_ra, b_rb])
            qsrc, b_qsrc = ra, b_ra
        else:
            qsrc, b_qsrc = qk, b_qk
        for grp in range(4):
            bank = 5 + grp % 2
            pT = K.psb[bank][:].rearrange("p (a b) -> p a b", b=128)

            def tr(e, grp=grp, pT=pT, qsrc=qsrc):
                for q in range(4):
                    i = grp * 4 + q
                    r = e.transpose(out=pT[:, q, :], in_=qsrc[:, i * 128:(i + 1) * 128], identity=C.ident[:])
                return r
            P.op("pe", tr, reads=[b_qsrc, C.b_ident], writes=[K.pbuf[bank]])
            P.op("dve", lambda e, grp=grp, pT=pT, s=s: e.tensor_copy(out=qT[s][:, grp * 4:(grp + 1) * 4, :], in_=pT[:, :, :]),
                 reads=[K.pbuf[bank]], writes=[b_qT[s]])

        def stq(e, t=t, s=s):
            return [e.dma_start(out=QT.ap()[t], in_=qT[s][:, 0:8, :]),
                    e.dma_start(out=KT.ap()[t], in_=qT[s][:, 8:16, :])]
        P.op("poolq", stq, reads=[b_qT[s]], ndma=2, key="qT%d" % s)
    P.barrier()
    K.sb_off = mark
    kth = [K.sb("kth%d" % i, [128, NT, 128], BF16) for i in range(2)]
    qth = [K.sb("qth%d" % i, [128, NT, 128], BF16) for i in range(2)]
    vh = [K.sb("vh%d" % i, [128, NT, 128], BF16) for i in range(2)]
    b_kqv = [Buf(), Buf()]
    NSL = 4
    pP = [K.sb("pP%d" % i, [128, 512], BF16) for i in range(NSL)]
    b_pP = [Buf() for _ in range(NSL)]
    onesb = K.sb("aonesb", [128, 128], BF16)
    b_onesb = Buf()
    P.op("dve", lambda e: e.memset(onesb[:], 1.0), writes=[b_onesb])
    qz = [K.sb("qz%d" % i, [128, 2, 256], BF16) for i in range(2)]
    b_qz = [Buf(), Buf()]

    def zq(e):
        for i in range(2):
            r = e.memset(qz[i][:], 0.0)
        return r
    P.op("pool", zq, writes=b_qz)
    rr = K.sb("rr", [128, 512], F32)
    on = K.sb("on", [128, 512], F32)
    oo = K.sb("oo", [128, 256], F32)
    r0 = K.sb("r0", [128, 256], F32)
    sqb = K.sb("sqb", [128, 256], F32)
    b_fin = Buf()
    b_sqb = Buf()
    b_AT = Buf()

    def ld_head(h, s):
        def f(e):
            return [e.dma_start(out=kth[s][:], in_=KT.ap()[:, :, h, :].rearrange("t p n -> p t n")),
                    e.dma_start(out=qth[s][:], in_=QT.ap()[:, :, h, :].rearrange("t p n -> p t n")),
                    e.dma_start(out=vh[s][:], in_=VS.ap()[:, :, h * 128:(h + 1) * 128].rearrange("t p d -> p t d"))]
        P.op("sp", f, writes=[b_kqv[s]], ndma=3, key="kqv%d" % s)
    ld_head(0, 0)
    qbn = 0
    scn = [0]
    pending = []
    DEPTH_ = 3
    for h in range(8):
        s = h % 2
        if h + 1 < 8:
            ld_head(h + 1, (h + 1) % 2)
        qblocks = [(0, list(range(0, 2)))] + [(2 + 2 * b, list(range(NT))) for b in range(16)]
        for (qt0, keys) in qblocks:
            nk = len(keys)
            pbo, pbs = (4, 5) if qbn % 2 == 0 else (6, 7)
            qsl = qbn % 2
            qbn += 1
            slots = {}

            def mkq(e, s=s, qt0=qt0, qsl=qsl):
                e.tensor_copy(out=qz[qsl][0:64, 0, :], in_=qth[s][0:64, qt0:qt0 + 2, :])
                return e.tensor_copy(out=qz[qsl][64:128, 1, :], in_=qth[s][64:128, qt0:qt0 + 2, :])
            P.op("pool", mkq, reads=[b_kqv[s]], writes=[b_qz[qsl]])

            def SC(ki, s=s, qt0=qt0, keys=keys, qsl=qsl):
                kt = keys[ki]
                sl = scn[0] % NSL
                scn[0] += 1
                slots[ki] = sl

                def f(e):
                    return e.matmul(K.psb[sl][:, :], lhsT=kth[s][:, kt, :], rhs=qz[qsl][:], start=True, stop=True)
                P.op("pe", f, reads=[b_kqv[s], b_qz[qsl]], writes=[K.pbuf[sl]])
                P.op("act", lambda e: e.activation(out=pP[sl][:], in_=K.psb[sl][:, :], func=AF.Exp, scale=0.125),
                     reads=[K.pbuf[sl]], writes=[b_pP[sl]])

            def PV(ki, s=s, keys=keys, nk=nk, pbo=pbo, pbs=pbs):
                kt = keys[ki]
                sl = slots[ki]

                def f(e):
                    st, sp_ = (ki == 0), (ki == nk - 1)
                    e.matmul(K.psb[pbo][:, :], lhsT=vh[s][:, kt, :], rhs=pP[sl][:], start=st, stop=sp_)
                    return e.matmul(K.psb[pbs][:, :], lhsT=onesb[:], rhs=pP[sl][:], start=st, stop=sp_)
                P.op("pe", f, reads=[b_kqv[s], b_pP[sl], b_onesb], writes=[K.pbuf[pbo], K.pbuf[pbs]])
            for ki in range(nk + DEPTH_):
                if ki < nk:
                    SC(ki)
                if ki >= DEPTH_:
                    PV(ki - DEPTH_)
                if pending and ki >= 3 and (ki - 3) % 4 == 0:
                    pending.pop(0)()
            while pending:
                pending.pop(0)()

            def e1(pbo=pbo, pbs=pbs):
                P.chain("dve", [
                    lambda e: e.reciprocal(out=rr[:], in_=K.psb[pbs][:, :]),
                    lambda e: e.tensor_tensor(out=on[:], in0=K.psb[pbo][:, :], in1=rr[:], op=ALU.mult),
                    lambda e: e.scalar_tensor_tensor(out=oo[:], in0=on[:, 256:512], scalar=nlam, in1=on[:, 0:256], op0=ALU.mult, op1=ALU.add),
                ], reads=[K.pbuf[pbo], K.pbuf[pbs], b_lam], writes=[b_fin])

            def e2(pbs=pbs):
                P.op("act", lambda e: e.activation(out=sqb[:], in_=oo[:], func=AF.Square), reads=[b_fin], writes=[b_sqb])
                P.op("pe", lambda e: e.matmul(K.psb[pbs][:, 0:256], lhsT=C.ones[:], rhs=sqb[:], start=True, stop=True),
                     reads=[b_sqb, C.b_ones], writes=[K.pbuf[pbs]])

            def e3(pbs=pbs):
                P.op("act", lambda e: e.activation(out=r0[:], in_=K.psb[pbs][:, 0:256], func=AF.Sqrt, scale=1.0 / 128, bias=C.eps[:]),
                     reads=[K.pbuf[pbs], C.b_eps, b_fin], writes=[b_fin])

            def e4():
                P.chain("dve", [
                    lambda e: e.reciprocal(out=r0[:], in_=r0[:]),
                    lambda e: e.tensor_tensor(out=oo[:], in0=oo[:], in1=r0[:], op=ALU.mult),
                ], reads=[b_fin], writes=[b_fin])

            def e5(qt0=qt0, h=h):
                P.op("act", lambda e: e.activation(out=AT[:, h, qt0 * 128:qt0 * 128 + 256], in_=oo[:], func=AF.Copy, scale=gs[:]),
                     reads=[b_fin, b_lam], writes=[b_AT, b_fin])
            pending.extend([e1, e2, e3, e4, e5])
    while pending:
        pending.pop(0)()
    P.barrier()
    K.sb_off = mark
    wo = K.sb("wo", [128, 8, D], BF16)
    b_wo = Buf()
    P.op("poolq", lambda e: [e.dma_start(out=wo[:, k, :], in_=dr["attn_w_o"].ap()[0, k * 128:(k + 1) * 128, :]) for k in range(8)],
         writes=[b_wo], ndma=8, key="wo")
    m2 = [K.sb("am2_%d" % i, [128, D], F32) for i in range(2)]
    b_m2 = Buf()
    P.op("sp", lambda e: [e.dma_start(out=m2[xc][:], in_=bcast_rows(dr["modrow"], (layer * 2 + xc) * 2048, 1024)) for xc in range(2)],
         writes=[b_m2], ndma=2, key="am2")
    xt = [K.sb("axt%d" % i, [128, D], F32) for i in range(2)]
    xo = [K.sb("axo%d" % i, [128, D], F32) for i in range(2)]
    b_xt, b_xo = [Buf(), Buf()], [Buf(), Buf()]
    for t in range(NT):
        s = t % 2
        xc = 1 if t < 2 else 0
        pb = 4 * s

        def mmo(e, t=t, pb=pb):
            for hf in range(2):
                for h in range(8):
                    r = e.matmul(K.psb[pb + hf][:, :], lhsT=AT[:, h, t * 128:(t + 1) * 128], rhs=wo[:, h, hf * 512:(hf + 1) * 512],
                                 start=(h == 0), stop=(h == 7))
            return r
        P.op("pe", mmo, reads=[b_AT, b_wo], writes=[K.pbuf[pb], K.pbuf[pb + 1]])
        P.op("sp", lambda e, t=t, s=s: [e.dma_start(out=xt[s][:], in_=src[t * 128:(t + 1) * 128, :])], writes=[b_xt[s]], ndma=1, key="axt%d" % s)

        def r0f(e, s=s, pb=pb, xc=xc):
            for hf in range(2):
                r = e.tensor_tensor(out=xo[s][:, hf * 512:(hf + 1) * 512], in0=K.psb[pb + hf][:, :], in1=m2[xc][:, hf * 512:(hf + 1) * 512], op=ALU.mult)
            return r
        P.chain("dve", [r0f, lambda e, s=s: e.tensor_tensor(out=xo[s][:], in0=xo[s][:], in1=xt[s][:], op=ALU.add)],
                reads=[K.pbuf[pb], K.pbuf[pb + 1], b_m2, b_xt[s]], writes=[b_xo[s]])
        P.op("poolq", lambda e, t=t, s=s: [e.dma_start(out=dst[t * 128:(t + 1) * 128, :], in_=xo[s][:])], reads=[b_xo[s]], ndma=1, key="axo%d" % s)


MIXERS[2] = stage_attn


FULL_PLAN = ["mod", "mix0", "ffn0", "mix1", "ffn1", "mix2", "ffn2", "mix3", "ffn3", "final"]


def kernel(**inputs):
    inp = {k: np.asarray(v) for k, v in inputs.items()}
    res, _ = run_plan(inp, FULL_PLAN, ncores=8)
    return np.ascontiguousarray(np.stack([np.asarray(res.results[b]["y"], np.float32) for b in range(8)], axis=0))
```

```python
import contextlib
import numpy as np
import ml_dtypes
import concourse.bass as bass
import concourse.mybir as mybir
from concourse.bass_utils import run_bass_kernel_spmd

F32 = mybir.dt.float32
BF16 = mybir.dt.bfloat16
AF = mybir.ActivationFunctionType
ALU = mybir.AluOpType
AX = mybir.AxisListType

D = 1024
NCTX = 256
NX = 4096
NTOK = NCTX + NX
NT = NTOK // 128
DFF = 3584
NE = 8
SEM_EPOCH = 4000
COMPUTE = ("pe", "act", "dve", "pool")
ENGMAP = {"pe": "pe", "act": "act", "dve": "dve", "pool": "pool", "sp": "sp", "actq": "act", "poolq": "pool"}


class Buf:
    __slots__ = ("name", "last_w", "readers")

    def __init__(self, name=""):
        self.name = name
        self.last_w = None
        self.readers = []


class Op:
    __slots__ = ("eng", "fn", "reads", "writes", "ndma", "key", "deps", "idx", "needed", "ev", "xdeps")

    def __init__(self, eng, fn, reads, writes, ndma, key, xdeps):
        self.eng = eng
        self.fn = fn
        self.reads = reads
        self.writes = writes
        self.ndma = ndma
        self.key = key
        self.deps = []
        self.needed = False
        self.ev = None
        self.xdeps = xdeps


class Prog:
    def __init__(self):
        self.ops = []
        self.last_real = {}
        self.dma_since = []

    def op(self, eng, fn, reads=(), writes=(), ndma=0, key=None, xdeps=()):
        o = Op(eng, fn, tuple(reads), tuple(writes), ndma, key, tuple(xdeps))
        o.idx = len(self.ops)
        self.ops.append(o)
        if fn is not None:
            self.last_real[ENGMAP[eng]] = o
            if ndma:
                self.dma_since.append(o)
        return o

    def chain(self, eng, fns, reads=(), writes=()):
        cb = Buf()
        for f in fns:
            self.op(eng, f, reads=tuple(reads) + (cb,), writes=tuple(writes) + (cb,))

    def barrier(self):
        deps = list(self.last_real.values()) + list(self.dma_since)
        self.dma_since = []
        for s in ("pe", "act", "dve", "pool", "sp"):
            self.op(s, None, xdeps=deps)

    def analyze(self):
        for o in self.ops:
            deps = {}
            for b in o.reads:
                if b.last_w is not None:
                    deps[b.last_w.idx] = b.last_w
            for b in o.writes:
                if b.last_w is not None:
                    deps[b.last_w.idx] = b.last_w
                for r in b.readers:
                    deps[r.idx] = r
            for d in o.xdeps:
                deps[d.idx] = d
            deps.pop(o.idx, None)
            o.deps = list(deps.values())
            for b in o.reads:
                b.readers.append(o)
            for b in o.writes:
                b.last_w = o
                b.readers = []

    def emit(self, nc, final_wait_keys=()):
        self.analyze()
        streams = {"pe": [], "act": [], "dve": [], "pool": [], "sp": []}
        for o in self.ops:
            streams[ENGMAP[o.eng]].append(o)
        for o in self.ops:
            for d in o.deps:
                if d.eng == "pe" and o.eng == "pe":
                    continue
                d.needed = True
        ccount = {e: 0 for e in COMPUTE}
        dcount = {}
        sem_names = []
        for o in self.ops:
            if o.fn is None:
                continue
            if o.ndma:
                k = o.key
                dcount[k] = dcount.get(k, 0) + 16 * o.ndma
                o.ev = ("d:" + k, dcount[k])
            elif o.needed:
                e = o.eng
                ccount[e] += 1
                ep = (ccount[e] - 1) // SEM_EPOCH
                o.ev = ("c:%s:%d" % (e, ep), ccount[e] - ep * SEM_EPOCH)
            if o.ev is not None and o.ev[0] not in sem_names:
                sem_names.append(o.ev[0])
        self.final = dict(dcount)
        self.nsem = len(sem_names)
        with contextlib.ExitStack() as es:
            sems = {n: es.enter_context(nc.semaphore(n.replace(":", "_"))) for n in sem_names}
            block = es.enter_context(nc.Block())
            latest = {}
            waits_for = {}
            seen = {s: {} for s in streams}
            for o in self.ops:
                need = {}
                for d in o.deps:
                    if d.eng == "pe" and o.eng == "pe":
                        continue
                    sname, val = d.ev
                    if sname[0] == "d":
                        val = max(val, latest.get(sname, 0))
                    if need.get(sname, 0) < val:
                        need[sname] = val
                st = seen[ENGMAP[o.eng]]
                w = []
                for sname, val in need.items():
                    if st.get(sname, 0) >= val:
                        continue
                    st[sname] = val
                    w.append((sname, val))
                waits_for[o.idx] = w
                if o.ev is not None and o.ndma:
                    latest[o.ev[0]] = o.ev[1]

            def make(stream_name):
                ops = streams[stream_name]

                def body(eng):
                    for o in ops:
                        for sname, val in waits_for[o.idx]:
                            eng.wait_ge(sems[sname], val)
                        if o.fn is None:
                            continue
                        r = o.fn(eng)
                        if o.ndma:
                            assert len(r) == o.ndma, (len(r), o.ndma, o.key)
                            for ins in r:
                                ins.then_inc(sems[o.ev[0]], 16)
                        elif o.ev is not None:
                            r.then_inc(sems[o.ev[0]], 1)
                    if stream_name == "sp":
                        for k in final_wait_keys:
                            eng.wait_ge(sems["d:" + k], self.final[k])
                return body

            block.tensor(make("pe"))
            block.scalar(make("act"))
            block.vector(make("dve"))
            block.gpsimd(make("pool"))
            block.sync(make("sp"))


SB_BASE = 16512
SB_LIMIT = 229344


class KB:
    def __init__(self):
        self.nc = bass.Bass("TRN2", target_bir_lowering=False)
        self.P = Prog()
        self.sb_off = SB_BASE
        self.sb_mark = SB_BASE
        self.uid = 0
        self.ins = {}
        self.outs = {}
        self.psb = []
        self.pbuf = []

    def din(self, name, shape, dt=F32):
        t = self.nc.dram_tensor(name, list(shape), dt, kind="ExternalInput")
        self.ins[name] = (tuple(shape), dt)
        return t

    def dout(self, name, shape, dt=F32):
        t = self.nc.dram_tensor(name, list(shape), dt, kind="ExternalOutput")
        self.outs[name] = (tuple(shape), dt)
        return t

    def dscr(self, name, shape, dt=F32):
        return self.nc.dram_tensor(name, list(shape), dt, kind="Internal")

    def sb(self, name, shape, dt):
        esz = 4 if dt == F32 else 2
        n = 1
        for s in shape[1:]:
            n *= s
        nbytes = (n * esz + 63) // 64 * 64
        off = self.sb_off
        assert off + nbytes <= SB_LIMIT, ("SBUF overflow", name, off, nbytes)
        self.sb_off += nbytes
        self.uid += 1
        return self.nc.alloc_sbuf_tensor_at("%s_%d" % (name, self.uid), list(shape), dt, offset=off)

    def persist(self):
        self.sb_mark = self.sb_off

    def stage_reset(self):
        self.P.barrier()
        self.sb_off = self.sb_mark

    def init_psum(self):
        for i in range(8):
            self.psb.append(self.nc.alloc_psum_tensor("psb%d" % i, [128, 512], F32))
            self.pbuf.append(Buf("ps%d" % i))


def bcast_rows(t, off, n):
    return bass.AP(t, off, [[0, 128], [1, n]])


class Common:
    pass


def setup_common(K, dr):
    P = K.P
    C = Common()
    C.ident = K.sb("ident", [128, 128], F32)
    C.b_ident = Buf()
    P.op("sp", lambda e: [e.dma_start(out=C.ident[:], in_=dr["ident"].ap())], writes=[C.b_ident], ndma=1, key="ident")
    C.eps = K.sb("eps", [128, 1], F32)
    C.b_eps = Buf()
    P.op("dve", lambda e: e.memset(C.eps[:], 1e-6), writes=[C.b_eps])
    C.ones = K.sb("ones", [128, 128], F32)
    C.b_ones = Buf()
    P.op("dve", lambda e: e.memset(C.ones[:], 1.0), writes=[C.b_ones])
    C.pcol = K.sb("pcol", [128, dr["pcol_n"]], F32)
    C.b_pcol = Buf()
    P.op("sp", lambda e: [e.dma_start(out=C.pcol[:], in_=dr["pcol"].ap())], writes=[C.b_pcol], ndma=1, key="pcol")
    C.modcol = K.sb("modcol", [128, 4 * 96], F32)
    C.b_modcol = Buf()
    C.scol = K.sb("scol", [128, 4 * 2 * 2 * 8], F32)
    C.b_scol = Buf()
    C.bcol = K.sb("bcol", [128, 4 * 2 * 2 * 8], F32)
    return C


def stage_mod(K, C, dr):
    P = K.P
    nc = K.nc
    ccol = K.sb("ccol", [128, 16], F32)
    sc = K.sb("sc", [128, 16], F32)
    b_cc, b_sc = Buf(), Buf()
    P.op("sp", lambda e: [e.dma_start(out=ccol[:], in_=dr["ccol"].ap())], writes=[b_cc], ndma=1, key="ccol")
    P.op("act", lambda e: e.activation(out=sc[:], in_=ccol[:], func=AF.Silu), reads=[b_cc], writes=[b_sc])
    wm = [K.sb("wm%d" % i, [128, 6144], F32) for i in range(2)]
    b_wm = [Buf(), Buf()]
    acc_col = K.sb("acc_col", [128, 96], F32)
    acc_row = K.sb("acc_row", [2, 2048], F32)
    brow = K.sb("brow", [2, 2048], F32)
    bmc = K.sb("bmc", [128, 48], F32)
    b_acc_col, b_acc_row, b_brow, b_bmc = Buf(), Buf(), Buf(), Buf()
    pcolv = K.psb[0]
    prow = [K.psb[1 + j] for j in range(4)]
    w_mod = dr["w_mod"]
    it = 0
    for i in range(4):
        P.op("sp", lambda e, i=i: [e.dma_start(out=bmc[:], in_=dr["bmodcol"].ap()[i])], writes=[b_bmc], ndma=1, key="bmc")

        def ldb(e, i=i):
            r = []
            for xc in range(2):
                for j, which in enumerate((2, 5)):
                    r.append(e.dma_start(out=brow[xc:xc + 1, j * 1024:(j + 1) * 1024],
                                         in_=dr["b_mod"].ap()[i:i + 1, which * 1024:(which + 1) * 1024]))
            return r
        P.op("sp", ldb, writes=[b_brow], ndma=4, key="brow")
        for kc in range(8):
            s = it % 2
            it += 1
            P.op("sp", lambda e, i=i, kc=kc, s=s: [e.dma_start(out=wm[s][:], in_=w_mod.ap()[i, kc * 128:(kc + 1) * 128, :])],
                 writes=[b_wm[s]], ndma=1, key="wm%d" % s)

            def mmc(e, kc=kc, s=s):
                for fc in range(48):
                    r = e.matmul(pcolv[:, fc * 2:fc * 2 + 2], lhsT=wm[s][:, fc * 128:(fc + 1) * 128],
                                 rhs=sc[:, kc * 2:kc * 2 + 2], start=True, stop=True)
                return r
            P.op("pe", mmc, reads=[b_wm[s], b_sc], writes=[K.pbuf[0]])

            def mmr(e, kc=kc, s=s):
                for j, which in enumerate((2, 5)):
                    for h in range(2):
                        r = e.matmul(prow[j * 2 + h][0:2, :], lhsT=sc[:, kc * 2:kc * 2 + 2],
                                     rhs=wm[s][:, which * 1024 + h * 512: which * 1024 + (h + 1) * 512],
                                     start=True, stop=True)
                return r
            P.op("pe", mmr, reads=[b_wm[s], b_sc], writes=[K.pbuf[1], K.pbuf[2], K.pbuf[3], K.pbuf[4]])
            if kc == 0:
                P.op("dve", lambda e: e.tensor_copy(out=acc_col[:], in_=pcolv[:, 0:96]), reads=[K.pbuf[0]], writes=[b_acc_col])

                def cpr(e):
                    for q in range(4):
                        r = e.tensor_copy(out=acc_row[:, q * 512:(q + 1) * 512], in_=prow[q][0:2, :])
                    return r
                P.op("dve", cpr, reads=[K.pbuf[1], K.pbuf[2], K.pbuf[3], K.pbuf[4]], writes=[b_acc_row])
            else:
                P.op("dve", lambda e: e.tensor_tensor(out=acc_col[:], in0=pcolv[:, 0:96], in1=acc_col[:], op=ALU.add),
                     reads=[K.pbuf[0], b_acc_col], writes=[b_acc_col])

                def adr(e):
                    for q in range(4):
                        r = e.tensor_tensor(out=acc_row[:, q * 512:(q + 1) * 512], in0=prow[q][0:2, :],
                                            in1=acc_row[:, q * 512:(q + 1) * 512], op=ALU.add)
                    return r
                P.op("dve", adr, reads=[K.pbuf[1], K.pbuf[2], K.pbuf[3], K.pbuf[4], b_acc_row], writes=[b_acc_row])

        def fin_col(e, i=i):
            a3 = acc_col[:].rearrange("p (f x) -> p f x", x=2)
            m3 = C.modcol[:, i * 96:(i + 1) * 96].rearrange("p (f x) -> p f x", x=2)
            for xc in range(2):
                r = e.tensor_tensor(out=m3[:, :, xc], in0=a3[:, :, xc], in1=bmc[:], op=ALU.add)
            return r
        P.op("dve", fin_col, reads=[b_acc_col, b_bmc], writes=[C.b_modcol])
        P.op("dve", lambda e: e.tensor_tensor(out=acc_row[:], in0=acc_row[:], in1=brow[:], op=ALU.add),
             reads=[b_acc_row, b_brow], writes=[b_acc_row])
        P.op("sp", lambda e, i=i: [e.dma_start(out=dr["modrow"].ap()[i], in_=acc_row[:])], reads=[b_acc_row], ndma=1, key="modrow")

        def mk_sb(e, i=i):
            for sub in range(2):
                wsc, wsh = (1, 0) if sub == 0 else (4, 3)
                gcol = C.pcol[:, dr["pc"]["norm_g"] + (i * 2 + sub) * 8: dr["pc"]["norm_g"] + (i * 2 + sub) * 8 + 8]
                for xc in range(2):
                    o = ((i * 2 + sub) * 2 + xc) * 8
                    msc = C.modcol[:, i * 96 + wsc * 16: i * 96 + wsc * 16 + 16].rearrange("p (f x) -> p f x", x=2)[:, :, xc]
                    msh = C.modcol[:, i * 96 + wsh * 16: i * 96 + wsh * 16 + 16].rearrange("p (f x) -> p f x", x=2)[:, :, xc]
                    e.scalar_tensor_tensor(out=C.scol[:, o:o + 8], in0=msc, scalar=1.0, in1=gcol, op0=ALU.add, op1=ALU.mult)
                    r = e.tensor_copy(out=C.bcol[:, o:o + 8], in_=msh)
            return r
        P.op("dve", mk_sb, reads=[C.b_modcol, C.b_pcol], writes=[C.b_scol])


class NMT:
    def __init__(self, K, C, want32=False, scale_eng="act", ev_eng="act", pbanks=(0, 1), nslots=2):
        self.K, self.C = K, C
        self.ns = nslots
        self.xt = [K.sb("xt%d" % i, [128, D], F32) for i in range(nslots)]
        self.xs = [K.sb("xs%d" % i, [128, D], F32) for i in range(nslots)]
        self.junk = K.sb("junk", [128, D], BF16)
        self.ss = [K.sb("ss%d" % i, [128, 1], F32) for i in range(nslots)]
        self.rs = [K.sb("rs%d" % i, [128, 1], F32) for i in range(nslots)]
        self.b_xt = [Buf() for _ in range(nslots)]
        self.b_xs = [Buf() for _ in range(nslots)]
        self.b_junk = Buf()
        self.b_ss = [Buf() for _ in range(nslots)]
        self.b_rs = [Buf() for _ in range(nslots)]
        self.n = 0
        self.want32 = want32
        self.scale_eng = scale_eng
        self.ev_eng = ev_eng
        self.pbanks = pbanks

    def tile(self, src_rows, layer, sub, is_ctx, out_ap_fn, out_buf, out32_fn=None, out32_buf=None):
        K, C, P = self.K, self.C, self.K.P
        s = self.n % self.ns
        self.n += 1
        xt, xs, ss, rs = self.xt[s], self.xs[s], self.ss[s], self.rs[s]
        P.op("sp", lambda e: [e.dma_start(out=xt[:], in_=src_rows)], writes=[self.b_xt[s]], ndma=1, key="xt%d" % s)
        P.op("act", lambda e: e.activation(out=self.junk[:], in_=xt[:], func=AF.Square, accum_out=ss[:]),
             reads=[self.b_xt[s]], writes=[self.b_junk, self.b_ss[s]])
        P.op("act", lambda e: e.activation(out=ss[:], in_=ss[:], func=AF.Sqrt, scale=1.0 / D, bias=C.eps[:]),
             reads=[self.b_ss[s], C.b_eps], writes=[self.b_ss[s]])
        P.op("dve", lambda e: e.reciprocal(out=rs[:], in_=ss[:]), reads=[self.b_ss[s]], writes=[self.b_rs[s]])
        if self.scale_eng == "act":
            P.op("act", lambda e: e.activation(out=xs[:], in_=xt[:], func=AF.Copy, scale=rs[:]),
                 reads=[self.b_xt[s], self.b_rs[s]], writes=[self.b_xs[s]])
        else:
            P.op(self.scale_eng, lambda e: e.tensor_scalar(out=xs[:], in0=xt[:], scalar1=rs[:], scalar2=None, op0=ALU.mult),
                 reads=[self.b_xt[s], self.b_rs[s]], writes=[self.b_xs[s]])
        o = ((layer * 2 + sub) * 2 + (1 if is_ctx else 0)) * 8
        for half in range(2):
            bank = self.pbanks[half]
            pT = K.psb[bank][:].rearrange("p (a b) -> p a b", b=128)

            def tr(e, half=half, pT=pT):
                for j in range(4):
                    f = half * 4 + j
                    r = e.transpose(out=pT[:, j, :], in_=xs[:, f * 128:(f + 1) * 128], identity=C.ident[:])
                return r
            P.op("pe", tr, reads=[self.b_xs[s], C.b_ident], writes=[K.pbuf[bank]])

            def ev(e, half=half, pT=pT):
                for j in range(4):
                    f = half * 4 + j
                    dsts = [out_ap_fn(f)] + ([out32_fn(f)] if out32_fn is not None else [])
                    for dd in dsts:
                        if self.ev_eng == "act":
                            r = e.activation(out=dd, in_=pT[:, j, :], func=AF.Identity,
                                             scale=C.scol[:, o + f:o + f + 1], bias=C.bcol[:, o + f:o + f + 1])
                        else:
                            r = e.tensor_scalar(out=dd, in0=pT[:, j, :], scalar1=C.scol[:, o + f:o + f + 1],
                                                scalar2=C.bcol[:, o + f:o + f + 1], op0=ALU.mult, op1=ALU.add)
                return r
            wr = [out_buf] + ([out32_buf] if out32_buf is not None else [])
            P.op(self.ev_eng, ev, reads=[K.pbuf[bank], C.b_scol], writes=wr)


def groups_of(tiles, gmax):
    ng = (len(tiles) + gmax - 1) // gmax
    base, rem = divmod(len(tiles), ng)
    out, i = [], 0
    for g in range(ng):
        n = base + (1 if g < rem else 0)
        out.append(tiles[i:i + n])
        i += n
    return out


def stage_ffn(K, C, dr, layer, src, dst, tiles, moe):
    P = K.P
    j = layer // 2
    E = NE if moe else 1
    GMAX = 12
    nmt = NMT(K, C, want32=moe, scale_eng="act", ev_eng="act", pbanks=(0, 1), nslots=3)
    TMAX = GMAX * 128
    hT = K.sb("hT", [128, 8, TMAX], BF16)
    yacc = K.sb("yacc", [128, GMAX, D], F32)
    actT = K.sb("actT", [128, 4, TMAX], BF16)
    wg = [K.sb("wg%d" % i, [128, 8, 512], BF16) for i in range(2)]
    wu = [K.sb("wu%d" % i, [128, 8, 512], BF16) for i in range(2)]
    wd = [K.sb("wd%d" % i, [128, 4, D], BF16) for i in range(2)]
    sg = [K.sb("sg%d" % i, [128, 512], F32) for i in range(2)]
    b_w = [Buf(), Buf()]
    b_sg = [Buf(), Buf()]
    m5 = [K.sb("m5_%d" % i, [128, D], F32) for i in range(2)]
    b_m5 = Buf()
    xo = [K.sb("xo%d" % i, [128, D], F32) for i in range(2)]
    b_xo = [Buf(), Buf()]

    def ldm5(e):
        return [e.dma_start(out=m5[xc][:], in_=bcast_rows(dr["modrow"], (layer * 2 + xc) * 2048 + 1024, 1024)) for xc in range(2)]
    P.op("sp", ldm5, writes=[b_m5], ndma=2, key="m5")
    if moe:
        h32 = K.sb("h32", [128, 8, 128], F32)
        b_h32 = Buf()
        wr = K.sb("wr", [128, 8, NE], F32)
        b_wr = Buf()
        P.op("sp", lambda e: [e.dma_start(out=wr[:], in_=dr["moe_w_router"].ap()[j].rearrange("(k p) e -> p k e", p=128))],
             writes=[b_wr], ndma=1, key="wr")
        gates = K.sb("gates", [128, GMAX, NE], F32)
        lg = K.sb("lg", [128, NE], F32)
        eq1 = K.sb("eq1", [128, NE], F32)
        eq2 = K.sb("eq2", [128, NE], F32)
        l2 = K.sb("l2", [128, NE], F32)
        mm = K.sb("mm", [128, 4], F32)
        b_rt = Buf()
    if moe:
        wG, wU, wD = dr["moe_w_gate"].ap()[j], dr["moe_w_up"].ap()[j], dr["moe_w_down"].ap()[j]
    else:
        wG, wU, wD = dr["ffn_w_gate"].ap()[j:j + 1], dr["ffn_w_up"].ap()[j:j + 1], dr["ffn_w_down"].ap()[j:j + 1]
    nslab = DFF // 512
    wcount = [0]
    dcount = [0]

    def load_w(e_idx, s_idx):
        slot = wcount[0] % 2
        wcount[0] += 1

        def f(e, slot=slot):
            r = []
            for k in range(8):
                r.append(e.dma_start(out=wg[slot][:, k, :], in_=wG[e_idx, k * 128:(k + 1) * 128, s_idx * 512:(s_idx + 1) * 512]))
                r.append(e.dma_start(out=wu[slot][:, k, :], in_=wU[e_idx, k * 128:(k + 1) * 128, s_idx * 512:(s_idx + 1) * 512]))
            for k in range(4):
                r.append(e.dma_start(out=wd[slot][:, k, :], in_=wD[e_idx, s_idx * 512 + k * 128: s_idx * 512 + (k + 1) * 128, :]))
            return r
        P.op("poolq", f, writes=[b_w[slot]], ndma=20, key="w%d_L%d" % (slot, layer))
        return slot

    b_hT = [Buf() for _ in range(GMAX)]
    b_y = [Buf() for _ in range(GMAX)]
    b_g = [Buf() for _ in range(GMAX)]
    grps = groups_of(tiles, GMAX)

    def phase1_tile(grp, li):
        if True:
            t = grp[li]
            is_ctx = t < 2
            nmt.tile(src[t * 128:(t + 1) * 128, :], layer, 1, is_ctx,
                     lambda f, li=li: hT[:, f, li * 128:(li + 1) * 128], b_hT[li],
                     (lambda f: h32[:, f, :]) if moe else None, b_h32 if moe else None)
            if moe:
                plg = K.psb[2]

                def rmm(e):
                    for k in range(8):
                        r = e.matmul(plg[:, 0:NE], lhsT=h32[:, k, :], rhs=wr[:, k, :], start=(k == 0), stop=(k == 7))
                    return r
                P.op("pe", rmm, reads=[b_h32, b_wr], writes=[K.pbuf[2]])

                P.chain("dve", [
                    lambda e: e.tensor_copy(out=lg[:], in_=plg[:, 0:NE]),
                    lambda e: e.tensor_reduce(out=mm[:, 0:1], in_=lg[:], axis=AX.X, op=ALU.max),
                    lambda e: e.tensor_scalar(out=eq1[:], in0=lg[:], scalar1=mm[:, 0:1], scalar2=None, op0=ALU.is_equal),
                    lambda e: e.scalar_tensor_tensor(out=l2[:], in0=eq1[:], scalar=-1e30, in1=lg[:], op0=ALU.mult, op1=ALU.add),
                    lambda e: e.tensor_reduce(out=mm[:, 1:2], in_=l2[:], axis=AX.X, op=ALU.max),
                    lambda e: e.tensor_scalar(out=eq2[:], in0=l2[:], scalar1=mm[:, 1:2], scalar2=None, op0=ALU.is_equal),
                    lambda e: e.tensor_tensor(out=mm[:, 2:3], in0=mm[:, 1:2], in1=mm[:, 0:1], op=ALU.subtract),
                ], reads=[K.pbuf[2]], writes=[b_rt])
                P.op("act", lambda e: e.activation(out=mm[:, 2:3], in_=mm[:, 2:3], func=AF.Sigmoid), reads=[b_rt], writes=[b_rt])
                P.chain("dve", [
                    lambda e: e.tensor_scalar(out=mm[:, 3:4], in0=mm[:, 2:3], scalar1=-1.0, scalar2=1.0, op0=ALU.mult, op1=ALU.add),
                    lambda e, li=li: e.tensor_scalar(out=gates[:, li, :], in0=eq1[:], scalar1=mm[:, 3:4], scalar2=None, op0=ALU.mult),
                    lambda e, li=li: e.scalar_tensor_tensor(out=gates[:, li, :], in0=eq2[:], scalar=mm[:, 2:3], in1=gates[:, li, :],
                                                            op0=ALU.mult, op1=ALU.add),
                ], reads=[b_rt], writes=[b_g[li], b_rt])
    def phase3_tile(grp, li):
        t = grp[li]
        xc = 1 if t < 2 else 0
        s = nmt.n % nmt.ns
        nmt.n += 1
        xt = nmt.xt[s]
        P.op("sp", lambda e: [e.dma_start(out=xt[:], in_=src[t * 128:(t + 1) * 128, :])],
             writes=[nmt.b_xt[s]], ndma=1, key="xt%d" % s)
        so = li % 2
        P.chain("dve", [
            lambda e: e.tensor_tensor(out=xo[so][:], in0=yacc[:, li, :], in1=m5[xc][:], op=ALU.mult),
            lambda e: e.tensor_tensor(out=xo[so][:], in0=xo[so][:], in1=xt[:], op=ALU.add),
        ], reads=[b_y[li], b_m5, nmt.b_xt[s]], writes=[b_xo[so]])
        P.op("poolq", lambda e: [e.dma_start(out=dst[t * 128:(t + 1) * 128, :], in_=xo[so][:])],
             reads=[b_xo[so]], ndma=1, key="xo%d" % so)

    for li in range(len(grps[0])):
        phase1_tile(grps[0], li)
    for gi, grp in enumerate(grps):
        ntl = len(grp)
        blocks = [list(range(ntl))[i:i + 4] for i in range(0, ntl, 4)]
        b_act = [Buf() for _ in blocks]
        seq = [(e_, s_) for e_ in range(E) for s_ in range(nslab)]
        if gi == 0:
            slot_next = load_w(*seq[0])
        for qi, (e_, s_) in enumerate(seq):
            slot = slot_next
            if qi + 1 < len(seq):
                slot_next = load_w(*seq[qi + 1])
            elif gi + 1 < len(grps):
                slot_next = load_w(*seq[0])
            first = (qi == 0)

            def GU(bi, slot=slot):
                blk = blocks[bi]
                t0, n = blk[0] * 128, len(blk) * 128
                for c in range(4):
                    pg, pu = 2 + (c % 2), 4 + (c % 2)

                    def mmg(e, c=c, pg=pg, w=wg):
                        for k in range(8):
                            r = e.matmul(K.psb[pg][:, 0:n], lhsT=w[slot][:, k, c * 128:(c + 1) * 128], rhs=hT[:, k, t0:t0 + n],
                                         start=(k == 0), stop=(k == 7))
                        return r
                    P.op("pe", mmg, reads=[b_w[slot]] + [b_hT[li] for li in blk], writes=[K.pbuf[pg]])

                    def mmu(e, c=c, pu=pu):
                        for k in range(8):
                            r = e.matmul(K.psb[pu][:, 0:n], lhsT=wu[slot][:, k, c * 128:(c + 1) * 128], rhs=hT[:, k, t0:t0 + n],
                                         start=(k == 0), stop=(k == 7))
                        return r
                    P.op("pe", mmu, reads=[b_w[slot]] + [b_hT[li] for li in blk], writes=[K.pbuf[pu]])
                    P.op("act", lambda e, c=c, pg=pg: e.activation(out=sg[c % 2][:, 0:n], in_=K.psb[pg][:, 0:n], func=AF.Silu),
                         reads=[K.pbuf[pg]], writes=[b_sg[c % 2]])
                    P.op("dve", lambda e, c=c, pu=pu: e.tensor_tensor(out=actT[:, c, t0:t0 + n], in0=K.psb[pu][:, 0:n],
                                                                     in1=sg[c % 2][:, 0:n], op=ALU.mult),
                         reads=[K.pbuf[pu], b_sg[c % 2]], writes=[b_act[bi]])

            def DN(bi, slot=slot, first=first, e_=e_):
                blk = blocks[bi]
                for li in blk:
                    dcount[0] += 1
                    pd = (0, 1) if dcount[0] % 2 == 0 else (6, 7)

                    def mmd(e, li=li, pd=pd):
                        for h in range(2):
                            for c in range(4):
                                r = e.matmul(K.psb[pd[h]][:, :], lhsT=actT[:, c, li * 128:(li + 1) * 128],
                                             rhs=wd[slot][:, c, h * 512:(h + 1) * 512], start=(c == 0), stop=(c == 3))
                        return r
                    P.op("pe", mmd, reads=[b_w[slot], b_act[bi]], writes=[K.pbuf[pd[0]], K.pbuf[pd[1]]])

                    def acc(e, li=li, pd=pd):
                        for h in range(2):
                            o = yacc[:, li, h * 512:(h + 1) * 512]
                            if moe:
                                gcol = gates[:, li, e_:e_ + 1]
                                if first:
                                    r = e.tensor_scalar(out=o, in0=K.psb[pd[h]][:, :], scalar1=gcol, scalar2=None, op0=ALU.mult)
                                else:
                                    r = e.scalar_tensor_tensor(out=o, in0=K.psb[pd[h]][:, :], scalar=gcol, in1=o,
                                                               op0=ALU.mult, op1=ALU.add)
                            else:
                                if first:
                                    r = e.tensor_copy(out=o, in_=K.psb[pd[h]][:, :])
                                else:
                                    r = e.tensor_tensor(out=o, in0=K.psb[pd[h]][:, :], in1=o, op=ALU.add)
                        return r
                    P.op("dve", acc, reads=[K.pbuf[pd[0]], K.pbuf[pd[1]]] + ([b_g[li]] if moe else []) + [b_y[li]], writes=[b_y[li]])

            nb = len(blocks)
            for bi in range(nb + 1):
                if bi < nb:
                    GU(bi)
                if bi >= 1:
                    DN(bi - 1)
        nxt = grps[gi + 1] if gi + 1 < len(grps) else []
        for li in range(max(ntl, len(nxt))):
            if li < ntl:
                phase3_tile(grp, li)
            if li < len(nxt):
                phase1_tile(nxt, li)


def stage_final(K, C, dr, src, dst):
    P = K.P
    xt = [K.sb("fxt%d" % i, [128, D], F32) for i in range(2)]
    xo = [K.sb("fxo%d" % i, [128, D], F32) for i in range(2)]
    junk = K.sb("fjunk", [128, D], BF16)
    ss = [K.sb("fss%d" % i, [128, 1], F32) for i in range(2)]
    gb = K.sb("fg", [128, D], F32)
    b_xt, b_xo, b_ss = [Buf(), Buf()], [Buf(), Buf()], [Buf(), Buf()]
    b_junk, b_g = Buf(), Buf()
    P.op("sp", lambda e: [e.dma_start(out=gb[:], in_=bcast_rows(dr["prow"], dr["pr"]["final_g"] * 1024, 1024))],
         writes=[b_g], ndma=1, key="fg")
    for t in range(2, NT):
        s = t % 2
        P.op("sp", lambda e, t=t, s=s: [e.dma_start(out=xt[s][:], in_=src[t * 128:(t + 1) * 128, :])],
             writes=[b_xt[s]], ndma=1, key="fxt%d" % s)
        P.op("act", lambda e, s=s: e.activation(out=junk[:], in_=xt[s][:], func=AF.Square, accum_out=ss[s][:]),
             reads=[b_xt[s]], writes=[b_junk, b_ss[s]])
        P.op("act", lambda e, s=s: e.activation(out=ss[s][:], in_=ss[s][:], func=AF.Sqrt, scale=1.0 / D, bias=C.eps[:]),
             reads=[b_ss[s], C.b_eps], writes=[b_ss[s]])
        P.op("dve", lambda e, s=s: e.reciprocal(out=ss[s][:], in_=ss[s][:]), reads=[b_ss[s]], writes=[b_ss[s]])
        P.op("dve", lambda e, s=s: e.scalar_tensor_tensor(out=xo[s][:], in0=xt[s][:], scalar=ss[s][:], in1=gb[:],
                                                           op0=ALU.mult, op1=ALU.mult),
             reads=[b_xt[s], b_ss[s], b_g], writes=[b_xo[s]])
        P.op("poolq", lambda e, t=t, s=s: [e.dma_start(out=dst[(t - 2) * 128:(t - 1) * 128, :], in_=xo[s][:])],
             reads=[b_xo[s]], ndma=1, key="fxo%d" % s)


def col_layout(v):
    v = np.asarray(v, np.float32).reshape(-1)
    return np.ascontiguousarray(v.reshape(-1, 128).T)


def pack_params(inp):
    pc, cols = {}, []
    n = 0

    def add(name, arr):
        nonlocal n
        a = col_layout(arr)
        pc[name] = n
        cols.append(a)
        n += a.shape[1]
    add("norm_g", np.concatenate([col_layout(inp["norm_g"][i, s_]) for i in range(4) for s_ in range(2)], axis=1).T.reshape(-1)
        if False else np.stack([inp["norm_g"][i, s_] for i in range(4) for s_ in range(2)]).reshape(-1))
    add("conv_b_in", inp["conv_b_in"].reshape(-1))
    add("conv_b_dw", inp["conv_b_dw"].reshape(-1))
    add("conv_ln_g", inp["conv_ln_g"].reshape(-1))
    add("conv_ln_b", inp["conv_ln_b"].reshape(-1))
    add("conv_w_dw", inp["conv_w_dw"].reshape(-1))
    add("attn_subln_g", inp["attn_subln_g"].reshape(-1))
    pcol = np.ascontiguousarray(np.concatenate(cols, axis=1))
    pr = {"conv_b_out": 0, "fnet_b": 2, "final_g": 3}
    prow = np.ascontiguousarray(np.concatenate([inp["conv_b_out"].reshape(2, D), inp["fnet_b"].reshape(1, D),
                                                inp["final_g"].reshape(1, D)], axis=0).astype(np.float32))
    return pcol, pc, prow, pr


BIG = ["w_mod", "b_mod", "conv_w_in", "conv_w_out", "fnet_w", "attn_w_qkv", "attn_w_o", "attn_lambda",
       "ffn_w_gate", "ffn_w_up", "ffn_w_down", "moe_w_router", "moe_w_gate", "moe_w_up", "moe_w_down"]


def needed_inputs(plan):
    need = {"ident", "pcol", "prow", "ccol", "xin"}
    for st in plan:
        if st == "mod":
            need |= {"w_mod", "b_mod", "bmodcol"}
        elif st in ("ffn0", "ffn2"):
            need |= {"ffn_w_gate", "ffn_w_up", "ffn_w_down"}
        elif st in ("ffn1", "ffn3"):
            need |= {"moe_w_router", "moe_w_gate", "moe_w_up", "moe_w_down"}
        elif st in ("mix0", "mix3"):
            need |= {"conv_w_in", "conv_w_out"}
        elif st == "mix1":
            need |= {"fnet_w", "dft128", "dftx", "dftc"}
        elif st == "mix2":
            need |= {"attn_w_qkv", "attn_w_o", "attn_lambda", "rope"}
    return need


def host_shared(inp, need):
    sh = {}
    pcol, pc, prow, pr = pack_params(inp)
    sh["pcol"], sh["prow"] = pcol, prow
    sh["ident"] = np.eye(128, dtype=np.float32)
    for k in BIG:
        if k in need:
            sh[k] = np.ascontiguousarray(np.asarray(inp[k], np.float32))
    if "dft128" in need:
        sh["dft128"], sh["dftx"], sh["dftc"] = dft_tables()
    if "rope" in need:
        sh["rope"] = rope_table()
    if "bmodcol" in need:
        sh["bmodcol"] = np.ascontiguousarray(np.asarray(inp["b_mod"], np.float32).reshape(4, 48, 128).transpose(0, 2, 1))
    return sh, pc, pr


def host_core(inp, b):
    m = {}
    m["xin"] = np.ascontiguousarray(np.concatenate([inp["ctx"][b], inp["x"][b]], axis=0).astype(np.float32))
    cc = np.zeros((128, 16), np.float32)
    cc[:, 0::2] = col_layout(inp["c"][b])
    cc[:, 1::2] = col_layout(inp["c_ctx"])
    m["ccol"] = cc
    return m


def build(plan, shapes, pc, pr, modcol_in=False):
    K = KB()
    need = needed_inputs(plan)
    dr = {"pc": pc, "pr": pr}
    for name in sorted(need):
        dr[name] = K.din(name, shapes[name][0], BF16 if shapes[name][1] == "bf16" else F32)
    dr["pcol_n"] = shapes["pcol"][0][1]
    dr["modrow"] = K.dscr("modrow", [4, 2, 2048])
    xs = K.dscr("xs", [NTOK, D])
    data = [st for st in plan if st != "mod"]
    fin = len(data) > 0 and data[-1] == "final"
    if fin:
        out = K.dout("y", [NX, D])
    else:
        out = K.dout("xout", [NTOK, D])
    K.init_psum()
    C = setup_common(K, dr)
    K.persist()
    for st in plan:
        if st == "mod":
            stage_mod(K, C, dr)
            K.stage_reset()
            continue
        i = data.index(st)
        src = dr["xin"].ap() if i == 0 else xs.ap()
        dst = out.ap() if i == len(data) - 1 else xs.ap()
        if st.startswith("ffn"):
            layer = int(st[3])
            tiles = list(range(NT)) if layer < 3 else list(range(2, NT))
            stage_ffn(K, C, dr, layer, src, dst, tiles, moe=(layer % 2 == 1))
        elif st.startswith("mix"):
            layer = int(st[3])
            MIXERS[layer % 3](K, C, dr, layer, src, dst)
        elif st == "final":
            stage_final(K, C, dr, src, dst)
        K.stage_reset()
    dma_keys = sorted({o.key for o in K.P.ops if o.ndma})
    K.P.emit(K.nc, final_wait_keys=dma_keys)
    return K


MIXERS = {}


def run_plan(inp, plan, ncores=8, trace=False):
    need = needed_inputs(plan)
    sh, pc, pr = host_shared(inp, need)
    in_maps = []
    for b in range(ncores):
        m = dict(sh)
        m.update(host_core(inp, b))
        in_maps.append({k: v for k, v in m.items() if k in need})
    shapes = {k: (v.shape, v.dtype.type if v.dtype != ml_dtypes.bfloat16 else "bf16") for k, v in in_maps[0].items()}
    K = build(plan, shapes, pc, pr)
    res = run_bass_kernel_spmd(K.nc, in_maps, core_ids=list(range(ncores)), trace=trace)
    return res, K


def stage_conv(K, C, dr, layer, src, dst):
    P = K.P
    j = layer // 3
    pc = dr["pc"]
    seqs = []
    if layer < 3:
        seqs.append((0, 2, True))
    seqs.append((2, 32, False))
    uT = {}
    for (t0, ntl, is_ctx) in seqs:
        L = ntl * 128
        uT[t0] = K.sb("uT%d" % t0, [128, 8, L + 30], BF16)
    mark = K.sb_off
    b_pad = Buf()

    def zero_pads(e):
        for (t0, ntl, is_ctx) in seqs:
            L = ntl * 128
            e.memset(uT[t0][:, :, 0:15], 0.0)
            r = e.memset(uT[t0][:, :, 15 + L:30 + L], 0.0)
        return r
    P.op("dve", zero_pads, writes=[b_pad])
    nmt = NMT(K, C, scale_eng="act", ev_eng="act", pbanks=(0, 1), nslots=3)
    win = K.sb("win", [128, 8, 2048], BF16)
    b_win = Buf()

    def ld_win(e):
        return [e.dma_start(out=win[:, k, :], in_=dr["conv_w_in"].ap()[j, k * 128:(k + 1) * 128, :]) for k in range(8)]
    P.op("poolq", ld_win, writes=[b_win], ndma=8, key="win")
    hTb = [K.sb("hTb%d" % i, [128, 8, 512], BF16) for i in range(2)]
    b_hTb = [Buf(), Buf()]
    sig = [K.sb("sig%d" % i, [128, 512], F32) for i in range(2)]
    b_sig = [Buf(), Buf()]
    b_u = Buf()
    bi_in = pc["conv_b_in"] + j * 16
    nblk = 0
    for (t0, ntl, is_ctx) in seqs:
        for b0 in range(0, ntl, 4):
            bt = list(range(b0, min(b0 + 4, ntl)))
            n = len(bt) * 128
            s = nblk % 2
            nblk += 1
            for li, t in enumerate(bt):
                nmt.tile(src[(t0 + t) * 128:(t0 + t + 1) * 128, :], layer, 0, is_ctx,
                         lambda f, li=li, s=s: hTb[s][:, f, li * 128:(li + 1) * 128], b_hTb[s])
            for c in range(8):
                pa, pg = 2 + (c % 2), 4 + (c % 2)

                def mma(e, c=c, pa=pa, s=s, n=n):
                    for k in range(8):
                        r = e.matmul(K.psb[pa][:, 0:n], lhsT=win[:, k, c * 128:(c + 1) * 128], rhs=hTb[s][:, k, 0:n],
                                     start=(k == 0), stop=(k == 7))
                    return r
                P.op("pe", mma, reads=[b_win, b_hTb[s]], writes=[K.pbuf[pa]])

                def mmg(e, c=c, pg=pg, s=s, n=n):
                    for k in range(8):
                        r = e.matmul(K.psb[pg][:, 0:n], lhsT=win[:, k, 1024 + c * 128:1024 + (c + 1) * 128], rhs=hTb[s][:, k, 0:n],
                                     start=(k == 0), stop=(k == 7))
                    return r
                P.op("pe", mmg, reads=[b_win, b_hTb[s]], writes=[K.pbuf[pg]])
                P.op("act", lambda e, c=c, pg=pg, n=n: e.activation(out=sig[c % 2][:, 0:n], in_=K.psb[pg][:, 0:n], func=AF.Sigmoid,
                                                                  bias=C.pcol[:, bi_in + 8 + c: bi_in + 9 + c]),
                     reads=[K.pbuf[pg], C.b_pcol], writes=[b_sig[c % 2]])
                P.op("dve", lambda e, c=c, pa=pa, n=n, t0=t0, b0=b0: e.scalar_tensor_tensor(
                    out=uT[t0][:, c, 15 + b0 * 128: 15 + b0 * 128 + n], in0=K.psb[pa][:, 0:n],
                    scalar=C.pcol[:, bi_in + c: bi_in + c + 1], in1=sig[c % 2][:, 0:n], op0=ALU.add, op1=ALU.mult),
                     reads=[K.pbuf[pa], b_sig[c % 2], C.b_pcol, b_pad], writes=[b_u])
    P.barrier()
    K.sb_off = mark
    NB = 256
    dg = K.sb("dg", [128, 8, 31, 128], BF16)
    b_dg = Buf()
    wdw = pc["conv_w_dw"] + j * 31 * 8

    def mk_dg(e):
        for c in range(8):
            for tap in range(31):
                r = e.tensor_scalar(out=dg[:, c, tap, :], in0=C.ident[:], scalar1=C.pcol[:, wdw + tap * 8 + c: wdw + tap * 8 + c + 1],
                                    scalar2=None, op0=ALU.mult)
        return r
    P.op("dve", mk_dg, reads=[C.b_ident, C.b_pcol], writes=[b_dg])
    wout = K.sb("wout", [128, 8, D], BF16)
    b_wout = Buf()
    P.op("poolq", lambda e: [e.dma_start(out=wout[:, k, :], in_=dr["conv_w_out"].ap()[j, k * 128:(k + 1) * 128, :]) for k in range(8)],
         writes=[b_wout], ndma=8, key="wout")
    eps5 = K.sb("eps5", [128, 1], F32)
    b_eps5 = Buf()
    P.op("dve", lambda e: e.memset(eps5[:], 1e-5), writes=[b_eps5])
    v32 = K.sb("v32", [128, 8, NB], F32)
    b_v = [Buf() for _ in range(8)]
    sq = [K.sb("sq%d" % i, [128, NB], F32) for i in range(2)]
    b_sq = [Buf(), Buf()]
    z = K.sb("z", [128, 8, NB], BF16)
    b_z = Buf()
    mean = K.sb("mean", [128, NB], F32)
    msq = K.sb("msq", [128, NB], F32)
    rstd = K.sb("rstd", [128, NB], F32)
    b_st = Buf()
    t1 = [K.sb("t1_%d" % i, [128, NB], F32) for i in range(2)]
    b_t1 = [Buf(), Buf()]
    xt = [K.sb("cxt%d" % i, [128, D], F32) for i in range(2)]
    xo = [K.sb("cxo%d" % i, [128, D], F32) for i in range(2)]
    b_xt, b_xo = [Buf(), Buf()], [Buf(), Buf()]
    m2 = [K.sb("m2_%d" % i, [128, D], F32) for i in range(2)]
    mb = [K.sb("mb_%d" % i, [128, D], F32) for i in range(2)]
    b_m2 = Buf()

    def ldm2(e):
        r = [e.dma_start(out=m2[xc][:], in_=bcast_rows(dr["modrow"], (layer * 2 + xc) * 2048, 1024)) for xc in range(2)]
        for xc in range(2):
            r.append(e.dma_start(out=mb[xc][:], in_=bcast_rows(dr["prow"], (dr["pr"]["conv_b_out"] + j) * 1024, 1024)))
        return r
    P.op("sp", ldm2, writes=[b_m2], ndma=4, key="m2")

    def mkmb(e):
        for xc in range(2):
            r = e.tensor_tensor(out=mb[xc][:], in0=m2[xc][:], in1=mb[xc][:], op=ALU.mult)
        return r
    P.op("dve", mkmb, reads=[b_m2], writes=[b_m2])
    cb_dw = pc["conv_b_dw"] + j * 8
    cg = pc["conv_ln_g"] + j * 8
    cbb = pc["conv_ln_b"] + j * 8
    ntile_out = 0
    for (t0, ntl, is_ctx) in seqs:
        L = ntl * 128
        xc = 1 if is_ctx else 0
        for p0 in range(0, L, NB):
            def conv(c, p0=p0, t0=t0):
                def f(e):
                    for tap in range(31):
                        r = e.matmul(K.psb[c % 2][:, 0:NB], lhsT=dg[:, c, tap, :], rhs=uT[t0][:, c, p0 + tap: p0 + tap + NB],
                                     start=(tap == 0), stop=(tap == 30))
                    return r
                P.op("pe", f, reads=[b_dg], writes=[K.pbuf[c % 2]])
                P.op("act", lambda e: e.activation(out=v32[:, c, :], in_=K.psb[c % 2][:, 0:NB], func=AF.Identity,
                                                   bias=C.pcol[:, cb_dw + c: cb_dw + c + 1]),
                     reads=[K.pbuf[c % 2], C.b_pcol], writes=[b_v[c]])
                P.op("act", lambda e: e.activation(out=sq[c % 2][:], in_=v32[:, c, :], func=AF.Square),
                     reads=[b_v[c]], writes=[b_sq[c % 2]])

            def stats(c):
                def f(e):
                    e.matmul(K.psb[2][:, 0:NB], lhsT=C.ones[:], rhs=v32[:, c, :], start=(c == 0), stop=(c == 7))
                    return e.matmul(K.psb[3][:, 0:NB], lhsT=C.ones[:], rhs=sq[c % 2][:], start=(c == 0), stop=(c == 7))
                P.op("pe", f, reads=[b_v[c], b_sq[c % 2], C.b_ones], writes=[K.pbuf[2], K.pbuf[3]])
            for c in range(9):
                if c < 8:
                    conv(c)
                if c >= 1:
                    stats(c - 1)

            P.chain("dve", [
                lambda e: e.tensor_scalar(out=mean[:], in0=K.psb[2][:, 0:NB], scalar1=1.0 / D, scalar2=None, op0=ALU.mult),
                lambda e: e.tensor_tensor(out=msq[:], in0=mean[:], in1=mean[:], op=ALU.mult),
                lambda e: e.scalar_tensor_tensor(out=rstd[:], in0=K.psb[3][:, 0:NB], scalar=1.0 / D, in1=msq[:],
                                                 op0=ALU.mult, op1=ALU.subtract),
            ], reads=[K.pbuf[2], K.pbuf[3]], writes=[b_st])
            P.op("act", lambda e: e.activation(out=rstd[:], in_=rstd[:], func=AF.Sqrt, bias=eps5[:]),
                 reads=[b_st, b_eps5], writes=[b_st])
            P.op("dve", lambda e: e.reciprocal(out=rstd[:], in_=rstd[:]), reads=[b_st], writes=[b_st])
            for c in range(8):
                P.chain("dve", [
                    lambda e, c=c: e.tensor_tensor(out=t1[c % 2][:], in0=v32[:, c, :], in1=mean[:], op=ALU.subtract),
                    lambda e, c=c: e.tensor_tensor(out=t1[c % 2][:], in0=t1[c % 2][:], in1=rstd[:], op=ALU.mult),
                ], reads=[b_v[c], b_st], writes=[b_t1[c % 2]])
                P.op("act", lambda e, c=c: e.activation(out=z[:, c, :], in_=t1[c % 2][:], func=AF.Silu,
                                                        scale=C.pcol[:, cg + c: cg + c + 1], bias=C.pcol[:, cbb + c: cbb + c + 1]),
                     reads=[b_t1[c % 2], C.b_pcol], writes=[b_z])
            for q in range(NB // 128):
                t = t0 + (p0 // 128) + q
                s = ntile_out % 2
                ntile_out += 1
                pb = 4 + 2 * s

                def mmo(e, q=q, pb=pb):
                    for h in range(2):
                        for c in range(8):
                            r = e.matmul(K.psb[pb + h][:, :], lhsT=z[:, c, q * 128:(q + 1) * 128], rhs=wout[:, c, h * 512:(h + 1) * 512],
                                         start=(c == 0), stop=(c == 7))
                    return r
                P.op("pe", mmo, reads=[b_z, b_wout], writes=[K.pbuf[pb], K.pbuf[pb + 1]])
                P.op("sp", lambda e, t=t, s=s: [e.dma_start(out=xt[s][:], in_=src[t * 128:(t + 1) * 128, :])],
                     writes=[b_xt[s]], ndma=1, key="cxt%d" % s)

                def res0(e, s=s, pb=pb, xc=xc):
                    for h in range(2):
                        r = e.tensor_tensor(out=xo[s][:, h * 512:(h + 1) * 512], in0=K.psb[pb + h][:, :],
                                            in1=m2[xc][:, h * 512:(h + 1) * 512], op=ALU.mult)
                    return r
                P.chain("dve", [
                    res0,
                    lambda e, s=s, xc=xc: e.tensor_tensor(out=xo[s][:], in0=xo[s][:], in1=mb[xc][:], op=ALU.add),
                    lambda e, s=s: e.tensor_tensor(out=xo[s][:], in0=xo[s][:], in1=xt[s][:], op=ALU.add),
                ], reads=[K.pbuf[pb], K.pbuf[pb + 1], b_m2, b_xt[s]], writes=[b_xo[s]])
                P.op("poolq", lambda e, t=t, s=s: [e.dma_start(out=dst[t * 128:(t + 1) * 128, :], in_=xo[s][:])],
                     reads=[b_xo[s]], ndma=1, key="cxo%d" % s)


MIXERS[0] = stage_conv


def dft_tables():
    d = np.arange(128)
    ang = 2 * np.pi * ((d[:, None] * d[None, :]) % 128) / 128.0
    t128 = np.concatenate([np.cos(ang), np.sin(ang)], axis=1) / np.sqrt(128.0)

    def seq_table(N):
        A = N // 128
        p = np.arange(128)
        a = np.arange(A)
        n = (a[None, :] * 128 + p[:, None])
        k = np.arange(N).reshape(A, 128)
        prod = (n[None, :, :, None].astype(np.int64) * k[:, None, None, :].astype(np.int64)) % N
        ang = prod.astype(np.float64) * (2 * np.pi / N)
        s = 1.0 / np.sqrt(N)
        tb = np.stack([np.cos(ang) * s, -np.sin(ang) * s], axis=2)
        return np.ascontiguousarray(tb.astype(np.float32).astype(ml_dtypes.bfloat16))
    return t128.astype(np.float32), seq_table(NX), seq_table(NCTX)


def stage_fnet(K, C, dr, layer, src, dst):
    P = K.P
    Hc = K.sb("Hc", [128, 32, D], BF16)
    Hs = K.sb("Hs", [128, 32, D], BF16)
    wf = K.sb("wf", [128, 8, D], BF16)
    t128 = K.sb("t128", [128, 256], BF16)
    m2s = K.sb("m2s", [128, D], F32)
    onesb = K.sb("onesb", [1, 128], BF16)
    fbb = K.sb("fbb", [1, D], BF16)
    b_wf, b_t128, b_m2s, b_ob = Buf(), Buf(), Buf(), Buf()
    P.op("poolq", lambda e: [e.dma_start(out=wf[:, k, :], in_=dr["fnet_w"].ap()[0, k * 128:(k + 1) * 128, :]) for k in range(8)],
         writes=[b_wf], ndma=8, key="wf")
    P.op("poolq", lambda e: [e.dma_start(out=t128[:], in_=dr["dft128"].ap()),
                             e.dma_start(out=fbb[:], in_=dr["prow"].ap()[dr["pr"]["fnet_b"]:dr["pr"]["fnet_b"] + 1, :])],
         writes=[b_t128, b_ob], ndma=2, key="t128")
    P.op("dve", lambda e: e.memset(onesb[:], 1.0), writes=[b_ob])
    mark = K.sb_off

    def do_seq(t0, ntl, is_ctx):
        xc = 1 if is_ctx else 0
        A = ntl
        K.sb_off = mark
        nmt = NMT(K, C, scale_eng="act", ev_eng="act", pbanks=(0, 1), nslots=3)
        hT = [K.sb("fhT%d" % i, [128, 8, 128], BF16) for i in range(2)]
        b_hT = [Buf(), Buf()]
        b_H = [Buf() for _ in range(A)]
        def f_nmt(a):
            s = a % 2
            nmt.tile(src[(t0 + a) * 128:(t0 + a + 1) * 128, :], layer, 0, is_ctx,
                     lambda f, s=s: hT[s][:, f, :], b_hT[s])
        f_nmt(0)
        for a in range(A):
            s = a % 2
            if a + 1 < A:
                f_nmt(a + 1)

            def mmh(e, s=s):
                for g in range(8):
                    bank = 2 + g // 2
                    r = e.matmul(K.psb[bank][:, (g % 2) * 256:(g % 2) * 256 + 256], lhsT=hT[s][:, g, :], rhs=t128[:],
                                 start=True, stop=True)
                return r
            P.op("pe", mmh, reads=[b_hT[s], b_t128], writes=[K.pbuf[2], K.pbuf[3], K.pbuf[4], K.pbuf[5]])

            def evh(e, a=a):
                for b in range(4):
                    v = K.psb[2 + b][:].rearrange("p (g c q) -> p g c q", g=2, c=2)
                    e.tensor_copy(out=Hc[:, a, b * 256:(b + 1) * 256].rearrange("p (g q) -> p g q", g=2), in_=v[:, :, 0, :])
                    r = e.tensor_copy(out=Hs[:, a, b * 256:(b + 1) * 256].rearrange("p (g q) -> p g q", g=2), in_=v[:, :, 1, :])
                return r
            P.op("dve", evh, reads=[K.pbuf[2], K.pbuf[3], K.pbuf[4], K.pbuf[5]], writes=[b_H[a]])
        P.barrier()
        K.sb_off = mark
        tb = [K.sb("tb%d" % i, [128, 2, A, 128], BF16) for i in range(2)]
        b_tb = [Buf(), Buf()]
        fsb = K.sb("fsb", [128, D], F32)
        fT = [K.sb("fT%d" % i, [128, 8, 128], BF16) for i in range(2)]
        xt0 = K.sb("nxt0", [128, D], F32)
        xt = [xt0, xt0]
        xo = K.sb("nxo", [128, D], F32)
        bx0 = Buf()
        b_fsb, b_fT, b_xt, b_xo = Buf(), [Buf(), Buf()], [bx0, bx0], Buf()
        P.op("sp", lambda e, xc=xc: [e.dma_start(out=m2s[:], in_=bcast_rows(dr["modrow"], (layer * 2 + xc) * 2048, 1024))],
             writes=[b_m2s], ndma=1, key="m2s")
        tbl = dr["dftc"] if is_ctx else dr["dftx"]

        def ld_tb(kc, s):
            P.op("sp", lambda e: [e.dma_start(out=tb[s][:], in_=tbl.ap()[kc])], writes=[b_tb[s]], ndma=1, key="tb%d" % s)
        ld_tb(0, 0)
        for kc in range(A):
            s = kc % 2
            if kc + 1 < A:
                ld_tb(kc + 1, (kc + 1) % 2)
            fb = (0, 1) if s == 0 else (6, 7)

            def mmf(e, s=s, fb=fb):
                for h in range(2):
                    i = 0
                    for a in range(A):
                        for cs, Hx in ((0, Hc), (1, Hs)):
                            r = e.matmul(K.psb[fb[h]][:, :], lhsT=tb[s][:, cs, a, :], rhs=Hx[:, a, h * 512:(h + 1) * 512],
                                         start=(i == 0), stop=(i == 2 * A - 1))
                            i += 1
                return r
            P.op("pe", mmf, reads=[b_tb[s]] + b_H, writes=[K.pbuf[fb[0]], K.pbuf[fb[1]]])

            def evf(e, fb=fb):
                for h in range(2):
                    r = e.activation(out=fsb[:, h * 512:(h + 1) * 512], in_=K.psb[fb[h]][:, :], func=AF.Copy)
                return r
            P.op("act", evf, reads=[K.pbuf[fb[0]], K.pbuf[fb[1]]], writes=[b_fsb])
            for half in range(2):
                pT = K.psb[2 + half][:].rearrange("p (a b) -> p a b", b=128)

                def tr(e, half=half, pT=pT):
                    for q in range(4):
                        f = half * 4 + q
                        r = e.transpose(out=pT[:, q, :], in_=fsb[:, f * 128:(f + 1) * 128], identity=C.ident[:])
                    return r
                P.op("pe", tr, reads=[b_fsb, C.b_ident], writes=[K.pbuf[2 + half]])
                P.op("dve", lambda e, half=half, pT=pT, s=s: e.tensor_copy(out=fT[s][:, half * 4:(half + 1) * 4, :], in_=pT[:, :, :]),
                     reads=[K.pbuf[2 + half]], writes=[b_fT[s]])

            def mmo(e, s=s):
                for h in range(2):
                    for c in range(8):
                        e.matmul(K.psb[4 + h][:, :], lhsT=fT[s][:, c, :], rhs=wf[:, c, h * 512:(h + 1) * 512],
                                 start=(c == 0), stop=False)
                    r = e.matmul(K.psb[4 + h][:, :], lhsT=onesb[0:1, :], rhs=fbb[0:1, h * 512:(h + 1) * 512], start=False, stop=True)
                return r
            P.op("pe", mmo, reads=[b_fT[s], b_wf, b_ob], writes=[K.pbuf[4], K.pbuf[5]])
            t = t0 + kc
            P.op("sp", lambda e, t=t, s=s: [e.dma_start(out=xt[s][:], in_=src[t * 128:(t + 1) * 128, :])],
                 writes=[b_xt[s]], ndma=1, key="nxt%d" % s)

            def r0(e):
                for h in range(2):
                    r = e.tensor_tensor(out=xo[:, h * 512:(h + 1) * 512], in0=K.psb[4 + h][:, :], in1=m2s[:, h * 512:(h + 1) * 512], op=ALU.mult)
                return r
            P.chain("dve", [r0, lambda e, s=s: e.tensor_tensor(out=xo[:], in0=xo[:], in1=xt[s][:], op=ALU.add)],
                    reads=[K.pbuf[4], K.pbuf[5], b_m2s, b_xt[s]], writes=[b_xo])
            import os
            if os.environ.get("FNET_DEBUG") and is_ctx:
                dbg = {"1": fsb, "2": m2s, "3": xt0}[os.environ["FNET_DEBUG"]]
                P.op("sp", lambda e, t=t, dbg=dbg: [e.dma_start(out=dst[t * 128:(t + 1) * 128, :], in_=dbg[:])], reads=[b_xo, b_fsb, b_m2s, bx0], ndma=1, key="nxo")
            else:
                P.op("poolq", lambda e, t=t: [e.dma_start(out=dst[t * 128:(t + 1) * 128, :], in_=xo[:])], reads=[b_xo], ndma=1, key="nxo")
        P.barrier()
    do_seq(0, 2, True)
    do_seq(2, 32, False)


MIXERS[1] = stage_fnet


def rope_table():
    rows = NX // 64
    row = np.repeat(np.arange(rows, dtype=np.float32), 64)
    col = np.tile(np.arange(64, dtype=np.float32), rows)
    inv = (1.0 / (10000.0 ** (np.arange(16, dtype=np.float32) * 2.0 / 32))).astype(np.float32)
    ang = np.stack([row[:, None] * inv, col[:, None] * inv], axis=1)
    ang = np.stack([ang, ang], axis=2).reshape(NX, 64).astype(np.float32)
    cos = np.cos(ang).astype(np.float32)
    sin = np.sin(ang).astype(np.float32).reshape(NX, 2, 2, 16).copy()
    sin[:, :, 0, :] *= -1.0
    return np.ascontiguousarray(np.concatenate([cos, sin.reshape(NX, 64)], axis=1).astype(np.float32))


def sb_bcast(t, off, pstep, dims):
    return bass.AP(t, off, [[pstep, 128]] + dims)


def stage_attn(K, C, dr, layer, src, dst):
    import math
    P = K.P
    lam_init = 0.8 - 0.6 * math.exp(-0.3 * layer)
    QT = K.dscr("QTs", [NT, 128, 8, 128], BF16)
    KT = K.dscr("KTs", [NT, 128, 8, 128], BF16)
    VS = K.dscr("VSs", [NT, 128, D], BF16)
    lv = K.sb("lv", [128, 256], F32)
    lt = K.sb("lt", [128, 128], F32)
    ls = K.sb("ls", [128, 4], F32)
    gs = K.sb("gs", [128, 1], F32)
    b_lv, b_lam = Buf(), Buf()
    P.op("sp", lambda e: [e.dma_start(out=lv[:], in_=bcast_rows(dr["attn_lambda"], 0, 256))], writes=[b_lv], ndma=1, key="lv")
    P.chain("dve", [
        lambda e: e.tensor_tensor(out=lt[:, 0:64], in0=lv[:, 0:64], in1=lv[:, 64:128], op=ALU.mult),
        lambda e: e.tensor_tensor(out=lt[:, 64:128], in0=lv[:, 128:192], in1=lv[:, 192:256], op=ALU.mult),
        lambda e: e.tensor_reduce(out=ls[:, 0:1], in_=lt[:, 0:64], axis=AX.X, op=ALU.add),
        lambda e: e.tensor_reduce(out=ls[:, 1:2], in_=lt[:, 64:128], axis=AX.X, op=ALU.add),
    ], reads=[b_lv], writes=[b_lam])
    P.op("act", lambda e: e.activation(out=ls[:, 0:2], in_=ls[:, 0:2], func=AF.Exp), reads=[b_lam], writes=[b_lam])
    sg_col = dr["pc"]["attn_subln_g"]
    P.chain("dve", [
        lambda e: e.tensor_tensor(out=ls[:, 2:3], in0=ls[:, 1:2], in1=ls[:, 0:1], op=ALU.subtract),
        lambda e: e.tensor_scalar(out=ls[:, 3:4], in0=ls[:, 2:3], scalar1=-lam_init, scalar2=None, op0=ALU.add),
        lambda e: e.tensor_scalar(out=gs[:], in0=C.pcol[:, sg_col:sg_col + 1], scalar1=1.0 - lam_init, scalar2=None, op0=ALU.mult),
    ], reads=[b_lam, C.b_pcol], writes=[b_lam])
    nlam = ls[:, 3:4]
    AT = K.sb("AT", [128, 8, NTOK], BF16)
    mark = K.sb_off
    nmt = NMT(K, C, scale_eng="act", ev_eng="act", pbanks=(0, 1))
    hT = [K.sb("ahT%d" % i, [128, 8, 128], BF16) for i in range(2)]
    b_hT = [Buf(), Buf()]
    wq = K.sb("wqkv", [128, 8, 3072], BF16)
    b_wq = Buf()
    P.op("poolq", lambda e: [e.dma_start(out=wq[:, k, :], in_=dr["attn_w_qkv"].ap()[0, k * 128:(k + 1) * 128, :]) for k in range(8)],
         writes=[b_wq], ndma=8, key="wqkv")
    qk_ = [K.sb("qk%d" % i, [128, 2048], F32) for i in range(2)]
    ra_ = [K.sb("ra%d" % i, [128, 2048], F32) for i in range(2)]
    rb = K.sb("rb", [128, 2048], F32)
    cs = [K.sb("cs%d" % i, [128, 128], F32) for i in range(2)]
    vb = [K.sb("vb%d" % i, [128, D], BF16) for i in range(2)]
    qT = [K.sb("qT%d" % i, [128, 16, 128], BF16) for i in range(2)]
    b_qk_, b_ra_, b_cs, b_vb, b_qT = [Buf(), Buf()], [Buf(), Buf()], [Buf(), Buf()], [Buf(), Buf()], [Buf(), Buf()]
    b_rb = Buf()

    def a_nmt(t):
        s = t % 2
        nmt.tile(src[t * 128:(t + 1) * 128, :], layer, 0, t < 2, lambda f, s=s: hT[s][:, f, :], b_hT[s])
    a_nmt(0)
    for t in range(NT):
        s = t % 2
        is_ctx = t < 2
        qk, ra, b_qk, b_ra = qk_[s], ra_[s], b_qk_[s], b_ra_[s]
        if t + 1 < NT:
            a_nmt(t + 1)
        def mmq(nb, bank, s=s):
            def mm(e):
                for k in range(8):
                    r = e.matmul(K.psb[bank][:, :], lhsT=hT[s][:, k, :], rhs=wq[:, k, nb * 512:(nb + 1) * 512], start=(k == 0), stop=(k == 7))
                return r
            P.op("pe", mm, reads=[b_hT[s], b_wq], writes=[K.pbuf[bank]])
        for nb in range(3):
            mmq(nb, 2 + nb)

        def evqkA(e, qk=qk):
            for nb in range(3):
                r = e.activation(out=qk[:, nb * 512:(nb + 1) * 512], in_=K.psb[2 + nb][:, :], func=AF.Copy)
            return r
        P.op("act", evqkA, reads=[K.pbuf[2], K.pbuf[3], K.pbuf[4]], writes=[b_qk])
        for nb in range(3, 6):
            mmq(nb, 2 + nb - 3)
        P.op("act", lambda e, qk=qk: e.activation(out=qk[:, 1536:2048], in_=K.psb[2][:, :], func=AF.Copy),
             reads=[K.pbuf[2]], writes=[b_qk])

        def evv(e, s=s):
            for nb in range(2):
                r = e.tensor_copy(out=vb[s][:, nb * 512:(nb + 1) * 512], in_=K.psb[3 + nb][:, :])
            return r
        P.op("dve", evv, reads=[K.pbuf[3], K.pbuf[4]], writes=[b_vb[s]])
        P.op("poolq", lambda e, t=t, s=s: [e.dma_start(out=VS.ap()[t], in_=vb[s][:])], reads=[b_vb[s]], ndma=1, key="vb%d" % s)
        if not is_ctx:
            P.op("sp", lambda e, t=t, s=s: [e.dma_start(out=cs[s][:], in_=dr["rope"].ap()[(t - 2) * 128:(t - 1) * 128, :])],
                 writes=[b_cs[s]], ndma=1, key="cs%d" % s)
            qk3 = qk[:].rearrange("p (g d) -> p g d", d=64)
            ra3 = ra[:].rearrange("p (g d) -> p g d", d=64)
            qk4 = qk[:].rearrange("p (g a h f) -> p g a h f", a=2, h=2, f=16)
            rb4 = rb[:].rearrange("p (g a h f) -> p g a h f", a=2, h=2, f=16)
            cosb = sb_bcast(cs[s], 0, 128, [[0, 32], [1, 64]])
            P.chain("dve", [
                lambda e, cosb=cosb, ra3=ra3, qk3=qk3: e.tensor_tensor(out=ra3, in0=qk3, in1=cosb, op=ALU.mult),
                lambda e, s=s, qk4=qk4: [e.tensor_tensor(out=rb4[:, :, ax, 0, :], in0=qk4[:, :, ax, 1, :],
                                                in1=sb_bcast(cs[s], 64 + ax * 32, 128, [[0, 32], [1, 16]]), op=ALU.mult) for ax in range(2)][-1],
                lambda e, s=s, qk4=qk4: [e.tensor_tensor(out=rb4[:, :, ax, 1, :], in0=qk4[:, :, ax, 0, :],
                                                in1=sb_bcast(cs[s], 64 + ax * 32 + 16, 128, [[0, 32], [1, 16]]), op=ALU.mult) for ax in range(2)][-1],
                lambda e, ra=ra: e.tensor_tensor(out=ra[:], in0=ra[:], in1=rb[:], op=ALU.add),
            ], reads=[b_qk, b_cs[s]], writes=[b_ra, b_rb])
            qsrc, b_qsrc = ra, b_ra
        else:
            qsrc, b_qsrc = qk, b_qk
        for grp in range(4):
            bank = 5 + grp % 2
            pT = K.psb[bank][:].rearrange("p (a b) -> p a b", b=128)

            def tr(e, grp=grp, pT=pT, qsrc=qsrc):
                for q in range(4):
                    i = grp * 4 + q
                    r = e.transpose(out=pT[:, q, :], in_=qsrc[:, i * 128:(i + 1) * 128], identity=C.ident[:])
                return r
            P.op("pe", tr, reads=[b_qsrc, C.b_ident], writes=[K.pbuf[bank]])
            P.op("dve", lambda e, grp=grp, pT=pT, s=s: e.tensor_copy(out=qT[s][:, grp * 4:(grp + 1) * 4, :], in_=pT[:, :, :]),
                 reads=[K.pbuf[bank]], writes=[b_qT[s]])

        def stq(e, t=t, s=s):
            return [e.dma_start(out=QT.ap()[t], in_=qT[s][:, 0:8, :]),
                    e.dma_start(out=KT.ap()[t], in_=qT[s][:, 8:16, :])]
        P.op("poolq", stq, reads=[b_qT[s]], ndma=2, key="qT%d" % s)
    P.barrier()
    K.sb_off = mark
    kth = [K.sb("kth%d" % i, [128, NT, 128], BF16) for i in range(2)]
    qth = [K.sb("qth%d" % i, [128, NT, 128], BF16) for i in range(2)]
    vh = [K.sb("vh%d" % i, [128, NT, 128], BF16) for i in range(2)]
    b_kqv = [Buf(), Buf()]
    NSL = 4
    pP = [K.sb("pP%d" % i, [128, 512], BF16) for i in range(NSL)]
    b_pP = [Buf() for _ in range(NSL)]
    onesb = K.sb("aonesb", [128, 128], BF16)
    b_onesb = Buf()
    P.op("dve", lambda e: e.memset(onesb[:], 1.0), writes=[b_onesb])
    qz = [K.sb("qz%d" % i, [128, 2, 256], BF16) for i in range(2)]
    b_qz = [Buf(), Buf()]

    def zq(e):
        for i in range(2):
            r = e.memset(qz[i][:], 0.0)
        return r
    P.op("pool", zq, writes=b_qz)
    rr = K.sb("rr", [128, 512], F32)
    on = K.sb("on", [128, 512], F32)
    oo = K.sb("oo", [128, 256], F32)
    r0 = K.sb("r0", [128, 256], F32)
    sqb = K.sb("sqb", [128, 256], F32)
    b_fin = Buf()
    b_sqb = Buf()
    b_AT = Buf()

    def ld_head(h, s):
        def f(e):
            return [e.dma_start(out=kth[s][:], in_=KT.ap()[:, :, h, :].rearrange("t p n -> p t n")),
                    e.dma_start(out=qth[s][:], in_=QT.ap()[:, :, h, :].rearrange("t p n -> p t n")),
                    e.dma_start(out=vh[s][:], in_=VS.ap()[:, :, h * 128:(h + 1) * 128].rearrange("t p d -> p t d"))]
        P.op("sp", f, writes=[b_kqv[s]], ndma=3, key="kqv%d" % s)
    ld_head(0, 0)
    qbn = 0
    scn = [0]
    pending = []
    DEPTH_ = 3
    for h in range(8):
        s = h % 2
        if h + 1 < 8:
            ld_head(h + 1, (h + 1) % 2)
        qblocks = [(0, list(range(0, 2)))] + [(2 + 2 * b, list(range(NT))) for b in range(16)]
        for (qt0, keys) in qblocks:
            nk = len(keys)
            pbo, pbs = (4, 5) if qbn % 2 == 0 else (6, 7)
            qsl = qbn % 2
            qbn += 1
            slots = {}

            def mkq(e, s=s, qt0=qt0, qsl=qsl):
                e.tensor_copy(out=qz[qsl][0:64, 0, :], in_=qth[s][0:64, qt0:qt0 + 2, :])
                return e.tensor_copy(out=qz[qsl][64:128, 1, :], in_=qth[s][64:128, qt0:qt0 + 2, :])
            P.op("pool", mkq, reads=[b_kqv[s]], writes=[b_qz[qsl]])

            def SC(ki, s=s, qt0=qt0, keys=keys, qsl=qsl):
                kt = keys[ki]
                sl = scn[0] % NSL
                scn[0] += 1
                slots[ki] = sl

                def f(e):
                    return e.matmul(K.psb[sl][:, :], lhsT=kth[s][:, kt, :], rhs=qz[qsl][:], start=True, stop=True)
                P.op("pe", f, reads=[b_kqv[s], b_qz[qsl]], writes=[K.pbuf[sl]])
                P.op("act", lambda e: e.activation(out=pP[sl][:], in_=K.psb[sl][:, :], func=AF.Exp, scale=0.125),
                     reads=[K.pbuf[sl]], writes=[b_pP[sl]])

            def PV(ki, s=s, keys=keys, nk=nk, pbo=pbo, pbs=pbs):
                kt = keys[ki]
                sl = slots[ki]

                def f(e):
                    st, sp_ = (ki == 0), (ki == nk - 1)
                    e.matmul(K.psb[pbo][:, :], lhsT=vh[s][:, kt, :], rhs=pP[sl][:], start=st, stop=sp_)
                    return e.matmul(K.psb[pbs][:, :], lhsT=onesb[:], rhs=pP[sl][:], start=st, stop=sp_)
                P.op("pe", f, reads=[b_kqv[s], b_pP[sl], b_onesb], writes=[K.pbuf[pbo], K.pbuf[pbs]])
            for ki in range(nk + DEPTH_):
                if ki < nk:
                    SC(ki)
                if ki >= DEPTH_:
                    PV(ki - DEPTH_)
                if pending and ki >= 3 and (ki - 3) % 4 == 0:
                    pending.pop(0)()
            while pending:
                pending.pop(0)()

            def e1(pbo=pbo, pbs=pbs):
                P.chain("dve", [
                    lambda e: e.reciprocal(out=rr[:], in_=K.psb[pbs][:, :]),
                    lambda e: e.tensor_tensor(out=on[:], in0=K.psb[pbo][:, :], in1=rr[:], op=ALU.mult),
                    lambda e: e.scalar_tensor_tensor(out=oo[:], in0=on[:, 256:512], scalar=nlam, in1=on[:, 0:256], op0=ALU.mult, op1=ALU.add),
                ], reads=[K.pbuf[pbo], K.pbuf[pbs], b_lam], writes=[b_fin])

            def e2(pbs=pbs):
                P.op("act", lambda e: e.activation(out=sqb[:], in_=oo[:], func=AF.Square), reads=[b_fin], writes=[b_sqb])
                P.op("pe", lambda e: e.matmul(K.psb[pbs][:, 0:256], lhsT=C.ones[:], rhs=sqb[:], start=True, stop=True),
                     reads=[b_sqb, C.b_ones], writes=[K.pbuf[pbs]])

            def e3(pbs=pbs):
                P.op("act", lambda e: e.activation(out=r0[:], in_=K.psb[pbs][:, 0:256], func=AF.Sqrt, scale=1.0 / 128, bias=C.eps[:]),
                     reads=[K.pbuf[pbs], C.b_eps, b_fin], writes=[b_fin])

            def e4():
                P.chain("dve", [
                    lambda e: e.reciprocal(out=r0[:], in_=r0[:]),
                    lambda e: e.tensor_tensor(out=oo[:], in0=oo[:], in1=r0[:], op=ALU.mult),
                ], reads=[b_fin], writes=[b_fin])

            def e5(qt0=qt0, h=h):
                P.op("act", lambda e: e.activation(out=AT[:, h, qt0 * 128:qt0 * 128 + 256], in_=oo[:], func=AF.Copy, scale=gs[:]),
                     reads=[b_fin, b_lam], writes=[b_AT, b_fin])
            pending.extend([e1, e2, e3, e4, e5])
    while pending:
        pending.pop(0)()
    P.barrier()
    K.sb_off = mark
    wo = K.sb("wo", [128, 8, D], BF16)
    b_wo = Buf()
    P.op("poolq", lambda e: [e.dma_start(out=wo[:, k, :], in_=dr["attn_w_o"].ap()[0, k * 128:(k + 1) * 128, :]) for k in range(8)],
         writes=[b_wo], ndma=8, key="wo")
    m2 = [K.sb("am2_%d" % i, [128, D], F32) for i in range(2)]
    b_m2 = Buf()
    P.op("sp", lambda e: [e.dma_start(out=m2[xc][:], in_=bcast_rows(dr["modrow"], (layer * 2 + xc) * 2048, 1024)) for xc in range(2)],
         writes=[b_m2], ndma=2, key="am2")
    xt = [K.sb("axt%d" % i, [128, D], F32) for i in range(2)]
    xo = [K.sb("axo%d" % i, [128, D], F32) for i in range(2)]
    b_xt, b_xo = [Buf(), Buf()], [Buf(), Buf()]
    for t in range(NT):
        s = t % 2
        xc = 1 if t < 2 else 0
        pb = 4 * s

        def mmo(e, t=t, pb=pb):
            for hf in range(2):
                for h in range(8):
                    r = e.matmul(K.psb[pb + hf][:, :], lhsT=AT[:, h, t * 128:(t + 1) * 128], rhs=wo[:, h, hf * 512:(hf + 1) * 512],
                                 start=(h == 0), stop=(h == 7))
            return r
        P.op("pe", mmo, reads=[b_AT, b_wo], writes=[K.pbuf[pb], K.pbuf[pb + 1]])
        P.op("sp", lambda e, t=t, s=s: [e.dma_start(out=xt[s][:], in_=src[t * 128:(t + 1) * 128, :])], writes=[b_xt[s]], ndma=1, key="axt%d" % s)

        def r0f(e, s=s, pb=pb, xc=xc):
            for hf in range(2):
                r = e.tensor_tensor(out=xo[s][:, hf * 512:(hf + 1) * 512], in0=K.psb[pb + hf][:, :], in1=m2[xc][:, hf * 512:(hf + 1) * 512], op=ALU.mult)
            return r
        P.chain("dve", [r0f, lambda e, s=s: e.tensor_tensor(out=xo[s][:], in0=xo[s][:], in1=xt[s][:], op=ALU.add)],
                reads=[K.pbuf[pb], K.pbuf[pb + 1], b_m2, b_xt[s]], writes=[b_xo[s]])
        P.op("poolq", lambda e, t=t, s=s: [e.dma_start(out=dst[t * 128:(t + 1) * 128, :], in_=xo[s][:])], reads=[b_xo[s]], ndma=1, key="axo%d" % s)


MIXERS[2] = stage_attn


FULL_PLAN = ["mod", "mix0", "ffn0", "mix1", "ffn1", "mix2", "ffn2", "mix3", "ffn3", "final"]


def kernel(**inputs):
    inp = {k: np.asarray(v) for k, v in inputs.items()}
    res, _ = run_plan(inp, FULL_PLAN, ncores=8)
    return np.ascontiguousarray(np.stack([np.asarray(res.results[b]["y"], np.float32) for b in range(8)], axis=0))
```
